# Optimizing a Trainium2 kernel written in Bass

```python
import math
import jax, jax.numpy as jnp
from jax import lax
import numpy as np

D_MODEL = 1024
BATCH = 8
SEQ = 8192
DEPTH = 1

HEAD_DIM = 64
MIX_WIDTH = D_MODEL
A_HEADS = MIX_WIDTH // (2 * HEAD_DIM)
A_KV_HEADS = max(1, A_HEADS // 4)
B_HEADS = MIX_WIDTH // HEAD_DIM - A_HEADS
A_WIDTH = A_HEADS * HEAD_DIM
A_KV_WIDTH = A_KV_HEADS * HEAD_DIM
B_WIDTH = B_HEADS * HEAD_DIM
IN_WIDTH = A_WIDTH + 2 * A_KV_WIDTH + 3 * B_WIDTH
IN_SPLITS = (A_WIDTH, A_WIDTH + A_KV_WIDTH, A_WIDTH + 2 * A_KV_WIDTH,
             A_WIDTH + 2 * A_KV_WIDTH + B_WIDTH, A_WIDTH + 2 * A_KV_WIDTH + 2 * B_WIDTH)
ROPE_THETA = 10000.0
GRID_W = 64
Q_BLOCK = 128
DILATED_PATTERNS = ((128, 1), (512, 4), (2048, 16))
SUBSEQ_BLOCK = 64
N_GROUPS = 4
EXPERTS_PER_GROUP = 8
N_EXPERTS = N_GROUPS * EXPERTS_PER_GROUP
TOP_K = 2
D_EXPERT = D_MODEL // 2
MOE_BLOCK = 512
EPS = 1e-6
NEG_INF = -1e30

kernel_name = "hymba_axial_gqa_longnet_hmoe_encoder"


def _rms_norm(x, g):
    xf = x.astype(jnp.float32)
    y = xf * lax.rsqrt(jnp.mean(xf * xf, axis=-1, keepdims=True) + EPS)
    return (y * g.astype(jnp.float32)).astype(x.dtype)


def _rope_inv_freq(dim):
    return 1.0 / (ROPE_THETA ** (jnp.arange(0, dim, 2, dtype=jnp.float32) / dim))


def _apply_rotary(x, angles):
    cos, sin = jnp.cos(angles), jnp.sin(angles)
    xf = x.astype(jnp.float32)
    x1, x2 = jnp.split(xf, 2, axis=-1)
    return jnp.concatenate([x1 * cos - x2 * sin, x2 * cos + x1 * sin], axis=-1).astype(x.dtype)


def _axial_angles(seq_len):
    rows = seq_len // GRID_W
    row = jnp.repeat(jnp.arange(rows, dtype=jnp.float32), GRID_W)
    col = jnp.tile(jnp.arange(GRID_W, dtype=jnp.float32), rows)
    f = _rope_inv_freq(HEAD_DIM // 2)
    return jnp.concatenate([row[:, None] * f, col[:, None] * f], axis=-1)


def _linear_angles(seq_len):
    t = jnp.arange(seq_len, dtype=jnp.float32)
    return t[:, None] * _rope_inv_freq(HEAD_DIM)


def _global_axial_gqa(q, k, v, q_norm_g, k_norm_g):
    b, s, _ = q.shape
    grp = A_HEADS // A_KV_HEADS
    q = q.reshape(b, s, A_KV_HEADS, grp, HEAD_DIM).transpose(0, 2, 3, 1, 4)
    k = k.reshape(b, s, A_KV_HEADS, HEAD_DIM).transpose(0, 2, 1, 3)
    v = v.reshape(b, s, A_KV_HEADS, HEAD_DIM).transpose(0, 2, 1, 3)
    ang = _axial_angles(s)
    q = _apply_rotary(_rms_norm(q, q_norm_g), ang)
    k = _apply_rotary(_rms_norm(k, k_norm_g), ang)
    nq = s // Q_BLOCK
    qb = jnp.moveaxis(q.reshape(b, A_KV_HEADS, grp, nq, Q_BLOCK, HEAD_DIM), 3, 0)
    scale = HEAD_DIM ** -0.5

    def block(qblk):
        sc = jnp.einsum('bkgqd,bksd->bkgqs', qblk, k, preferred_element_type=jnp.float32) * scale
        p = jax.nn.softmax(sc, axis=-1).astype(v.dtype)
        return jnp.einsum('bkgqs,bksd->bkgqd', p, v)

    o = lax.map(block, qb)
    o = jnp.moveaxis(o, 0, 3).reshape(b, A_KV_HEADS, grp, s, HEAD_DIM)
    return o.transpose(0, 3, 1, 2, 4).reshape(b, s, A_WIDTH)


def _dilated_window(q, k, v, window, dilation):
    b, h, s, d = q.shape
    r = window // (2 * dilation)
    L = s // dilation

    def to_sub(t):
        return t.reshape(b, h, L, dilation, d).transpose(0, 1, 3, 2, 4)

    qs, ks, vs = to_sub(q), to_sub(k), to_sub(v)
    nblk = -(-L // SUBSEQ_BLOCK)
    lp = nblk * SUBSEQ_BLOCK
    pad4 = ((0, 0), (0, 0), (0, 0))
    qs = jnp.pad(qs, pad4 + ((0, lp - L), (0, 0)))
    kpad = jnp.pad(ks, pad4 + ((r, lp - L + r), (0, 0)))
    vpad = jnp.pad(vs, pad4 + ((r, lp - L + r), (0, 0)))
    kwin = SUBSEQ_BLOCK + 2 * r
    kidx = jnp.arange(nblk)[:, None] * SUBSEQ_BLOCK + jnp.arange(kwin)[None, :]
    kb = jnp.take(kpad, kidx, axis=3)
    vb = jnp.take(vpad, kidx, axis=3)
    qb = qs.reshape(b, h, dilation, nblk, SUBSEQ_BLOCK, d)
    sc = jnp.einsum('bhrnqd,bhrnkd->bhrnqk', qb, kb, preferred_element_type=jnp.float32) * (d ** -0.5)
    qpos = jnp.arange(lp).reshape(nblk, SUBSEQ_BLOCK)
    kpos = kidx - r
    mask = (jnp.abs(qpos[:, :, None] - kpos[:, None, :]) <= r) & ((kpos >= 0) & (kpos < L))[:, None, :]
    sc = jnp.where(mask, sc, NEG_INF)
    m = jnp.max(sc, axis=-1, keepdims=True)
    p = jnp.exp(sc - m)
    den = jnp.sum(p, axis=-1, keepdims=True)
    o = jnp.einsum('bhrnqk,bhrnkd->bhrnqd', p.astype(vb.dtype), vb,
                   preferred_element_type=jnp.float32) / den
    lse = (m + jnp.log(den))[..., 0]
    o = o.astype(v.dtype).reshape(b, h, dilation, lp, d)[:, :, :, :L]
    o = o.transpose(0, 1, 3, 2, 4).reshape(b, h, s, d)
    lse = lse.reshape(b, h, dilation, lp)[:, :, :, :L].transpose(0, 1, 3, 2).reshape(b, h, s)
    return o, lse


def _longnet_mixture(q, k, v):
    b, s, _ = q.shape
    q = q.reshape(b, s, B_HEADS, HEAD_DIM).transpose(0, 2, 1, 3)
    k = k.reshape(b, s, B_HEADS, HEAD_DIM).transpose(0, 2, 1, 3)
    v = v.reshape(b, s, B_HEADS, HEAD_DIM).transpose(0, 2, 1, 3)
    ang = _linear_angles(s)
    q = _apply_rotary(q, ang)
    k = _apply_rotary(k, ang)
    outs, lses = [], []
    for window, dilation in DILATED_PATTERNS:
        o, lse = _dilated_window(q, k, v, window, dilation)
        outs.append(o)
        lses.append(lse)
    w = jax.nn.softmax(jnp.stack(lses, axis=0), axis=0).astype(v.dtype)
    o = jnp.einsum('pbhs,pbhsd->bhsd', w, jnp.stack(outs, axis=0))
    return o.transpose(0, 2, 1, 3).reshape(b, s, B_WIDTH)


def _hierarchical_moe(x, router_group_w, router_group_b, router_expert_w, router_expert_b,
                      w_gate, w_up, w_down):
    b, s, dm = x.shape
    n = b * s
    xt = x.reshape(n, dm)
    g_logits = (xt @ router_group_w).astype(jnp.float32) + router_group_b.astype(jnp.float32)
    g_prob = jax.nn.softmax(g_logits, axis=-1)
    g_top_p, g_top = lax.top_k(g_prob, 1)
    e_logits = (xt @ router_expert_w).astype(jnp.float32) + router_expert_b.astype(jnp.float32)
    e_logits = e_logits.reshape(n, N_GROUPS, EXPERTS_PER_GROUP)
    e_in_group = jnp.take_along_axis(e_logits, g_top[:, :, None], axis=1)[:, 0]
    e_top_logit, e_top = lax.top_k(e_in_group, TOP_K)
    gates = g_top_p * jax.nn.softmax(e_top_logit, axis=-1)
    expert_id = g_top * EXPERTS_PER_GROUP + e_top

    nk = n * TOP_K
    flat_e = expert_id.reshape(nk).astype(jnp.int32)
    flat_tok = jnp.arange(nk, dtype=jnp.int32) // TOP_K
    flat_gate = gates.reshape(nk)
    order = jnp.argsort(flat_e)
    se = flat_e[order]
    counts = jnp.bincount(flat_e, length=N_EXPERTS)
    start = jnp.cumsum(counts) - counts
    padded = (counts + MOE_BLOCK - 1) // MOE_BLOCK * MOE_BLOCK
    pend = jnp.cumsum(padded)
    pstart = pend - padded
    dest = pstart[se] + (jnp.arange(nk, dtype=jnp.int32) - start[se])
    n_blocks = -(-nk // MOE_BLOCK) + N_EXPERTS
    rows = n_blocks * MOE_BLOCK
    row_tok = jnp.zeros((rows,), jnp.int32).at[dest].set(flat_tok[order])
    row_gate = jnp.zeros((rows,), x.dtype).at[dest].set(flat_gate[order].astype(x.dtype))
    block_e = jnp.minimum(jnp.searchsorted(pend, jnp.arange(n_blocks) * MOE_BLOCK, side='right'),
                          N_EXPERTS - 1).astype(jnp.int32)

    def expert_block(args):
        e, tok = args
        xb = xt[tok]
        hdn = jax.nn.silu(xb @ w_gate[e]) * (xb @ w_up[e])
        return hdn @ w_down[e]

    yb = lax.map(expert_block, (block_e, row_tok.reshape(n_blocks, MOE_BLOCK)))
    y = jnp.zeros((n, dm), x.dtype).at[row_tok].add(yb.reshape(rows, dm) * row_gate[:, None])
    return y.reshape(b, s, dm)


def setup_inputs(seed: int = 0) -> dict:
    key = jax.random.key(seed)
    ks = jax.random.split(key, 20)
    f32 = jnp.float32

    def nrm(k, shape, scale):
        return jax.random.normal(k, shape, f32) * scale

    def gain(k, shape):
        return 1.0 + 0.02 * jax.random.normal(k, shape, f32)

    return {
        "x": jax.random.normal(ks[0], (BATCH, SEQ, D_MODEL), f32),
        "norm1_g": gain(ks[1], (DEPTH, D_MODEL)),
        "w_in": nrm(ks[2], (DEPTH, D_MODEL, IN_WIDTH), D_MODEL ** -0.5),
        "q_norm_g": gain(ks[3], (DEPTH, HEAD_DIM)),
        "k_norm_g": gain(ks[4], (DEPTH, HEAD_DIM)),
        "out_norm_a_g": gain(ks[5], (DEPTH, A_WIDTH)),
        "out_norm_b_g": gain(ks[6], (DEPTH, B_WIDTH)),
        "w_out": nrm(ks[7], (DEPTH, MIX_WIDTH, D_MODEL), MIX_WIDTH ** -0.5),
        "norm2_g": gain(ks[8], (DEPTH, D_MODEL)),
        "router_group_w": nrm(ks[9], (DEPTH, D_MODEL, N_GROUPS), D_MODEL ** -0.5),
        "router_group_b": nrm(ks[10], (DEPTH, N_GROUPS), 0.01),
        "router_expert_w": nrm(ks[11], (DEPTH, D_MODEL, N_EXPERTS), D_MODEL ** -0.5),
        "router_expert_b": nrm(ks[12], (DEPTH, N_EXPERTS), 0.01),
        "w_gate": nrm(ks[13], (DEPTH, N_EXPERTS, D_MODEL, D_EXPERT), D_MODEL ** -0.5),
        "w_up": nrm(ks[14], (DEPTH, N_EXPERTS, D_MODEL, D_EXPERT), D_MODEL ** -0.5),
        "w_down": nrm(ks[15], (DEPTH, N_EXPERTS, D_EXPERT, D_MODEL), D_EXPERT ** -0.5),
        "final_norm_g": gain(ks[16], (D_MODEL,)),
    }


def reference(x, norm1_g, w_in, q_norm_g, k_norm_g, out_norm_a_g, out_norm_b_g, w_out,
              norm2_g, router_group_w, router_group_b, router_expert_w, router_expert_b,
              w_gate, w_up, w_down, final_norm_g):
    h = x
    for l in range(DEPTH):
        u = _rms_norm(h, norm1_g[l])
        proj = u @ w_in[l]
        qa, ka, va, qb, kb, vb = jnp.split(proj, IN_SPLITS, axis=-1)
        oa = _rms_norm(_global_axial_gqa(qa, ka, va, q_norm_g[l], k_norm_g[l]), out_norm_a_g[l])
        ob = _rms_norm(_longnet_mixture(qb, kb, vb), out_norm_b_g[l])
        h = h + jnp.concatenate([oa, ob], axis=-1) @ w_out[l]
        h = h + _hierarchical_moe(_rms_norm(h, norm2_g[l]), router_group_w[l], router_group_b[l],
                                  router_expert_w[l], router_expert_b[l],
                                  w_gate[l], w_up[l], w_down[l])
    return _rms_norm(h, final_norm_g)
```

```python
import numpy as np
import concourse.bass as bass
import concourse.mybir as mybir
from concourse.bass_utils import run_bass_kernel_spmd

F32 = mybir.dt.float32
BF16 = mybir.dt.bfloat16
I32 = mybir.dt.int32
ALU = mybir.AluOpType
AF = mybir.ActivationFunctionType
AX = mybir.AxisListType

S = 8192
NT = 64
DM = 1024
CAP = 1024
CT = 32
RE = NT * CT
NE = 32
EPS = 1e-6
ENGS = ["tensor", "vector", "scalar", "gpsimd", "sync"]
NPOOL = 16
DEBUG = False


class KB:
    def __init__(self, nc):
        self.nc = nc
        self.ops = {e: [] for e in ENGS}
        self.last_w = {}
        self.readers = {}
        self.ndma = {e: 0 for e in ENGS}
        self.dmas = {e: [] for e in ENGS}

    def op(self, eng, fn, r=(), w=(), dma=False):
        rec = dict(eng=eng, fn=fn, deps=[], sig=False, dma=dma, idx=len(self.ops[eng]))
        deps = []
        for k in r:
            if k in self.last_w:
                deps.append(self.last_w[k])
        for k in w:
            if k in self.last_w:
                deps.append(self.last_w[k])
            deps.extend(self.readers.get(k, ()))
        for k in r:
            self.readers.setdefault(k, []).append(rec)
        for k in w:
            self.last_w[k] = rec
            self.readers[k] = []
        if dma:
            rec["k"] = self.ndma[eng]
            self.ndma[eng] += 1
            self.dmas[eng].append(rec)
        seen = set()
        for d in deps:
            if id(d) in seen or d is rec:
                continue
            seen.add(id(d))
            if d["eng"] == "tensor" and eng == "tensor" and not d["dma"]:
                continue
            d["sig"] = True
            rec["deps"].append(d)
        self.ops[eng].append(rec)
        return rec

    def barrier(self):
        lasts = []
        for e in ENGS:
            for rec in reversed(self.ops[e]):
                if rec["fn"] is not None and not rec["dma"]:
                    rec["sig"] = True
                    lasts.append(rec)
                    break
            lasts.extend(self.dmas[e][-NPOOL:])
        for e in ENGS:
            rec = dict(eng=e, fn=None, deps=list(lasts), sig=False, dma=False, idx=len(self.ops[e]))
            self.ops[e].append(rec)
        self.last_w = {}
        self.readers = {}

    def emit(self, sems, pools):
        nc = self.nc
        for e in ENGS:
            c = 0
            for rec in self.ops[e]:
                if rec["dma"]:
                    k = rec["k"]
                    rec["sem"] = pools[e][k % NPOOL]
                    rec["val"] = 16 * (k // NPOOL + 1)
                elif rec["sig"]:
                    c += 1
                    rec["sem"] = sems[e]
                    rec["val"] = c
        with nc.Block() as block:
            for e in ENGS:
                def mk(e):
                    def body(eng):
                        seen = {}
                        def wait(sem, val):
                            key = id(sem)
                            if seen.get(key, 0) >= val:
                                return
                            seen[key] = val
                            eng.wait_ge(sem, val)
                        for rec in self.ops[e]:
                            for d in rec["deps"]:
                                wait(d["sem"], d["val"])
                            if rec["fn"] is None:
                                continue
                            if rec["dma"]:
                                k = rec["k"]
                                if k >= NPOOL:
                                    wait(pools[e][k % NPOOL], 16 * (k // NPOOL))
                                rec["fn"](eng).then_inc(rec["sem"], 16)
                            else:
                                ins = rec["fn"](eng)
                                if rec["sig"]:
                                    ins.then_inc(rec["sem"], 1)
                    return body
                getattr(block, e)(mk(e))

    def mm(self, out, lhsT, rhs, start, stop, r=(), w=()):
        return self.op("tensor", lambda e: e.matmul(out, lhsT=lhsT, rhs=rhs, start=start, stop=stop), r, w)

    def tr(self, out, in_, ident, r=(), w=()):
        return self.op("tensor", lambda e: e.transpose(out, in_, ident), r, w)

    def act(self, out, in_, func, r=(), w=(), **kw):
        return self.op("scalar", lambda e: e.activation(out=out, in_=in_, func=func, **kw), r, w)

    def tt(self, eng, out, in0, in1, op, r=(), w=()):
        return self.op(eng, lambda e: e.tensor_tensor(out=out, in0=in0, in1=in1, op=op), r, w)

    def ts(self, eng, out, in0, s1, s2, op0, op1=None, r=(), w=()):
        if op1 is None:
            return self.op(eng, lambda e: e.tensor_scalar(out=out, in0=in0, scalar1=s1, scalar2=None, op0=op0), r, w)
        return self.op(eng, lambda e: e.tensor_scalar(out=out, in0=in0, scalar1=s1, scalar2=s2, op0=op0, op1=op1), r, w)

    def stt(self, eng, out, in0, scalar, in1, op0, op1, r=(), w=()):
        return self.op(eng, lambda e: e.scalar_tensor_tensor(out=out, in0=in0, scalar=scalar, in1=in1, op0=op0, op1=op1), r, w)

    def copy(self, eng, out, in_, r=(), w=()):
        if eng == "scalar":
            return self.op(eng, lambda e: e.activation(out=out, in_=in_, func=AF.Copy), r, w)
        return self.op(eng, lambda e: e.tensor_copy(out=out, in_=in_), r, w)

    def red(self, eng, out, in_, op, r=(), w=()):
        return self.op(eng, lambda e: e.tensor_reduce(out=out, in_=in_, axis=AX.X, op=op), r, w)

    def memset(self, eng, ap, val, r=(), w=()):
        return self.op(eng, lambda e: e.memset(ap, val), r, w)

    def dma(self, q, out, in_, r=(), w=()):
        return self.op(q, lambda e: e.dma_start(out=out, in_=in_), r, w, dma=True)

    def scatter(self, out, off, in_, bound, r=(), w=()):
        return self.op("gpsimd", lambda e: e.indirect_dma_start(
            out=out, out_offset=bass.IndirectOffsetOnAxis(ap=off, axis=0), in_=in_, in_offset=None,
            bounds_check=bound, oob_is_err=False), r, w, dma=True)

    def gather(self, out, in_, off, bound, r=(), w=()):
        return self.op("gpsimd", lambda e: e.indirect_dma_start(
            out=out, out_offset=None, in_=in_, in_offset=bass.IndirectOffsetOnAxis(ap=off, axis=0),
            bounds_check=bound, oob_is_err=False), r, w, dma=True)


def build_nc(debug=False, phases=6):
    nc = bass.Bass("TRN2", target_bir_lowering=False)
    dk = "ExternalOutput" if debug else "Internal"

    def din(name, shape, dt=F32):
        return nc.dram_tensor(name, list(shape), dt, kind="ExternalInput")

    x_d = din("x", [S, DM])
    w_in_d = din("w_in", [DM, 2304]); w_out_d = din("w_out", [DM, DM])
    g1_d = din("g1", [128, 8]); gab_d = din("gab", [128, 8]); g2_d = din("g2", [DM]); gf_d = din("gf", [DM])
    gq_d = din("gq", [64]); gk_d = din("gk", [64])
    rw_d = din("rw", [DM, 36]); rb_d = din("rb", [36])
    wg_d = din("w_gate", [NE, DM, 512]); wu_d = din("w_up", [NE, DM, 512]); wd_d = din("w_down", [NE, 512, DM])
    tabs_d = din("tabs", [S, 128])
    cid_d = din("c_ident", [128, 128]); ctri_d = din("c_triu", [128, 128]); cmask_d = din("c_mask", [128, 512])
    ce_d = din("c_e1024", [NE])
    cio_d = din("c_iota", [CT])
    out_d = nc.dram_tensor("out", [S, DM], F32, kind="ExternalOutput")

    QBs = nc.dram_tensor("QBs", [512, S], BF16, kind="Internal")
    KBs = nc.dram_tensor("KBs", [512, S], BF16, kind="Internal")
    VBs = nc.dram_tensor("VBs", [S + 2048, 520], BF16, kind="Internal")
    OT = nc.dram_tensor("OT", [DM, S], BF16, kind=dk)
    Hs = nc.dram_tensor("Hs", [S, DM], F32, kind=dk)
    XN = nc.dram_tensor("XN", [S, DM], BF16, kind="Internal")
    XS = nc.dram_tensor("XS", [NE, RE, DM], BF16, kind="Internal")
    Ys = nc.dram_tensor("Ys", [NE, RE, DM], F32, kind="Internal")
    LG = nc.dram_tensor("LG", [128, NT * 36], F32, kind=dk)
    DST = nc.dram_tensor("DST", [128, NT * NE], F32, kind=dk)

    def A(name, shape, dt):
        t_ = nc.alloc_sbuf_tensor(name, shape, dt)
        return t_[tuple(slice(None) for _ in shape)]
    identb = A("identb", [128, 128], BF16); identf = A("identf", [128, 128], F32)
    maskAB = A("maskAB", [128, 512], BF16); triU = A("triU", [128, 128], BF16)
    onesb = A("onesb", [128, 128], BF16); onesf = A("onesf", [128, 64], F32)
    g1c = A("g1c", [128, 8], F32); gabc = A("gabc", [128, 8], F32)
    gqk = A("gqk", [128, 12, 64], F32)
    g2bc = A("g2bc", [128, DM], F32); gfbc = A("gfbc", [128, DM], F32)
    rwsb = A("rwsb", [128, 8, 36], F32); rbbc = A("rbbc", [128, 36], F32)
    e1024 = A("e1024", [128, NE], F32)
    iota24 = A("iota24", [128, CT], F32)
    Gfull = A("Gfull", [128, NT, NE], F32)
    ARN = 94400
    arena = A("arena", [128, ARN], BF16)
    PS = nc.alloc_psum_tensor("PS", [128, 8, 512], F32)

    class Arena:
        def __init__(self):
            self.off = 0

        def reset(self, off=0):
            self.off = off

        def get(self, shape, dt):
            n = int(np.prod(shape[1:]))
            nb = n * (2 if dt == BF16 else 4)
            nb = (nb + 63) // 64 * 64
            a = self.off // 2
            self.off += nb
            assert self.off <= ARN * 2, ("arena overflow", self.off)
            v = arena[:, a:a + (n if dt == BF16 else 2 * n)]
            if dt != BF16:
                v = v.bitcast(dt)
            if len(shape) == 3:
                v = v.rearrange("p (a b) -> p a b", a=shape[1])
            elif len(shape) == 4:
                v = v.rearrange("p (a b c) -> p a b c", a=shape[1], b=shape[2])
            return v

    ar = Arena()
    k = KB(nc)
    epsc = A("epsc", [128, 1], F32)
    k.memset("vector", epsc[:], EPS, w=["epsc"])

    def rstd(out, in_, n, rk, wk):
        k.act(out, in_, AF.Sqrt, r=rk + ["epsc"], w=wk, scale=1.0 / n, bias=epsc[:, 0:1])
        k.op("vector", lambda e: e.reciprocal(out=out, in_=out), r=wk, w=wk)

    k.dma("gpsimd", identb[:], cid_d[:, :], w=["identb"])
    k.dma("sync", identf[:], cid_d[:, :], w=["identf"])
    k.dma("gpsimd", maskAB[:], cmask_d[:, :], w=["maskAB"])
    k.dma("gpsimd", triU[:], ctri_d[:, :], w=["triU"])
    k.memset("vector", onesb[:], 1.0, w=["onesb"])
    k.memset("vector", onesf[:], 1.0, w=["onesf"])
    k.dma("sync", g1c[:], g1_d[:, :], w=["g1c"])
    k.dma("sync", gabc[:], gab_d[:, :], w=["gabc"])
    for h in range(12):
        src = gq_d if h < 8 else gk_d
        k.dma("sync", gqk[:, h, :], src[:].partition_broadcast(128), w=["gqk"])
    k.ts("vector", gqk[:, 0:8, :], gqk[:, 0:8, :], 0.125, None, ALU.mult, r=["gqk"], w=["gqk"])
    k.dma("sync", g2bc[:], g2_d[:].partition_broadcast(128), w=["g2bc"])
    k.dma("sync", gfbc[:], gf_d[:].partition_broadcast(128), w=["gfbc"])
    k.dma("sync", rwsb[:], rw_d[:, :].rearrange("(k p) n -> p k n", p=128), w=["rwsb"])
    k.dma("sync", rbbc[:], rb_d[:].partition_broadcast(128), w=["rbbc"])
    k.dma("sync", e1024[:], ce_d[:].partition_broadcast(128), w=["e1024"])
    k.dma("sync", iota24[:], cio_d[:].partition_broadcast(128), w=["iota24"])

    win = ar.get([128, 8, 2432], BF16)
    QAT = ar.get([128, 4, S], BF16)
    KAT = ar.get([128, 2, S], BF16)
    VA = ar.get([128, NT, 130], BF16)
    base12 = ar.off

    wst = ar.get([128, 2304], F32)
    zt = ar.get([128, 8, 520], BF16)
    for kc in range(8):
        k.dma("sync", wst, w_in_d[kc * 128:(kc + 1) * 128, :], w=["wst"])
        gcol = g1c[:, kc:kc + 1]
        k.ts("vector", win[:, kc, 0:512], wst[:, 0:512], gcol, None, ALU.mult, r=["wst", "g1c"], w=["win"])
        k.ts("vector", win[:, kc, 512:2048], wst[:, 768:2304], gcol, None, ALU.mult, r=["wst", "g1c"], w=["win"])
        k.ts("gpsimd", win[:, kc, 2048:2304].rearrange("p (a b c) -> p a b c", a=2, b=2),
             wst[:, 512:640].rearrange("p (a b c) -> p a b c", a=2, b=1).broadcast_to([128, 2, 2, 64]),
             gcol, None, ALU.mult, r=["wst", "g1c"], w=["win"])
        k.ts("gpsimd", win[:, kc, 2304:2432], wst[:, 640:768], gcol, None, ALU.mult, r=["wst", "g1c"], w=["win"])
    k.memset("gpsimd", zt, 0.0, w=["zt"])
    k.dma("sync", VBs[0:1024, :].rearrange("(n p) c -> p n c", p=128), zt, r=["zt"])
    k.dma("sync", VBs[S + 1024:S + 2048, :].rearrange("(n p) c -> p n c", p=128), zt, r=["zt"])
    k.memset("gpsimd", VA.rearrange("p n (k d) -> p n k d", k=2)[:, :, :, 64:65], 1.0, w=["VA"])
    k.barrier()

    ar.reset(base12)
    xTb_ = ar.get([128, 8, 128], BF16); xTb = [xTb_, xTb_]
    xt = ar.get([128, DM], F32)
    junk = ar.get([128, DM], BF16)
    tab = [ar.get([128, 128], F32) for _ in range(2)]
    qk = ar.get([128, 12, 64], F32)
    sq = ar.get([128, 12, 64], F32)
    qkr = ar.get([128, 12, 64], BF16)
    t1 = ar.get([128, 12, 32], F32); t2 = ar.get([128, 12, 32], F32)
    qkb = ar.get([128, 16, 64], F32)
    qkbr = ar.get([128, 16, 64], BF16)
    u1 = ar.get([128, 16, 32], F32); u2 = ar.get([128, 16, 32], F32)
    vbst = [ar.get([128, 8, 65], BF16) for _ in range(2)]
    stT_ = ar.get([128, 8, 128], BF16); stT = [stT_, stT_]
    st1 = ar.get([128, 32], F32)
    for s_ in range(2):
        k.memset("gpsimd", vbst[s_][:, :, 64:65], 1.0, w=["vbst%d" % s_])

    def rotary(eng, src, dst, ta, tb, cs, sn, nh, kr, kw, pre):
        C = cs.unsqueeze(1).broadcast_to([128, nh, 32]); Sn = sn.unsqueeze(1).broadcast_to([128, nh, 32])
        x1 = src[:, :, 0:32]; x2 = src[:, :, 32:64]
        k.tt(eng, ta, x1, C, ALU.mult, r=kr, w=[pre + "a"])
        k.tt(eng, tb, x2, Sn, ALU.mult, r=kr, w=[pre + "b"])
        k.tt(eng, dst[:, :, 0:32], ta, tb, ALU.subtract, r=[pre + "a", pre + "b"], w=kw)
        k.tt(eng, ta, x2, C, ALU.mult, r=kr + kw, w=[pre + "a"])
        k.tt(eng, tb, x1, Sn, ALU.mult, r=kr + kw, w=[pre + "b"])
        k.tt(eng, dst[:, :, 32:64], ta, tb, ALU.add, r=[pre + "a", pre + "b"], w=kw)

    for t in range(NT if phases >= 1 else 0):
        s_ = t % 2
        tok = slice(t * 128, (t + 1) * 128)
        k.dma("sync", xt, x_d[tok, :], w=["xt"])
        k.dma("sync", tab[s_], tabs_d[tok, :], w=["tab%d" % s_])
        sqj = sq.rearrange("p h d -> p (h d)").bitcast(BF16)[:, 0:DM]
        k.act(sqj, xt, AF.Square, r=["xt"], w=["sq", "ss"], accum_out=st1[:, 0:1])
        k.copy("gpsimd", junk, xt, r=["xt"], w=["junk"])
        pX = PS[:, 7, :].bitcast(BF16).rearrange("p (c t) -> p c t", c=8)
        for kc in range(8):
            k.tr(pX[:, kc, :], junk[:, kc * 128:(kc + 1) * 128], identb[:], r=["junk", "identb"], w=["ps7"])
        k.copy("vector", xTb[s_], pX, r=["ps7"], w=["xTb"])
        rstd(st1[:, 1:2], st1[:, 0:1], DM, ["ss"], ["rstd1"])
        for b in range(5):
            n0, n1 = b * 512, min((b + 1) * 512, 2432)
            for kc in range(8):
                k.mm(PS[:, b, 0:n1 - n0], xTb[s_][:, kc, :], win[:, kc, n0:n1], kc == 0, kc == 7,
                     r=["xTb", "win"], w=["ps%d" % b])
        rs = st1[:, 1:2]
        k.act(qk[:, 0:8, :], PS[:, 0, :].rearrange("p (h d) -> p h d", h=8), AF.Copy, r=["ps0", "rstd1"], w=["qk"], scale=rs)
        k.act(qk[:, 8:12, :], PS[:, 4, 0:256].rearrange("p (h d) -> p h d", h=4), AF.Copy, r=["ps4", "rstd1"], w=["qk"], scale=rs)
        k.act(VA[:, t, :].rearrange("p (k d) -> p k d", k=2)[:, :, 0:64], PS[:, 4, 256:384].rearrange("p (k d) -> p k d", k=2),
              AF.Copy, r=["ps4", "rstd1"], w=["VA"], scale=rs)
        k.tt("vector", sq, qk, qk, ALU.mult, r=["qk"], w=["sq"])
        k.red("vector", st1[:, 2:14], sq, ALU.add, r=["sq"], w=["ssq"])
        rstd(st1[:, 14:26], st1[:, 2:14], 64, ["ssq"], ["rq"])
        k.tt("vector", qk, qk, st1[:, 14:26].unsqueeze(2).broadcast_to([128, 12, 64]), ALU.mult, r=["qk", "rq"], w=["qk"])
        k.tt("vector", qk, qk, gqk, ALU.mult, r=["qk", "gqk"], w=["qk"])
        rotary("vector", qk, qkr, t1, t2, tab[s_][:, 0:32], tab[s_][:, 32:64], 12, ["qk", "tab%d" % s_], ["qkr"], "t")
        k.act(qkb[:, 0:8, :], PS[:, 1, :].rearrange("p (h d) -> p h d", h=8), AF.Copy, r=["ps1", "rstd1"], w=["qkb"], scale=rs)
        k.act(qkb[:, 8:16, :], PS[:, 2, :].rearrange("p (h d) -> p h d", h=8), AF.Copy, r=["ps2", "rstd1"], w=["qkb"], scale=rs)
        k.ts("gpsimd", qkb[:, 0:8, :], qkb[:, 0:8, :], 0.125, None, ALU.mult, r=["qkb"], w=["qkb"])
        rotary("gpsimd", qkb, qkbr, u1, u2, tab[s_][:, 64:96], tab[s_][:, 96:128], 16, ["qkb", "tab%d" % s_], ["qkbr"], "u")
        k.act(vbst[s_][:, :, 0:64], PS[:, 3, :].rearrange("p (h d) -> p h d", h=8), AF.Copy,
              r=["ps3", "rstd1"], w=["vbst%d" % s_], scale=rs)
        k.dma("sync", VBs[1024 + t * 128:1024 + (t + 1) * 128, :], vbst[s_].rearrange("p h d -> p (h d)"),
              r=["vbst%d" % s_])
        pA = PS[:, 5, 0:384].bitcast(BF16).rearrange("p (c t) -> p c t", c=6)
        qkr2 = qkr.rearrange("p h d -> p (h d)")
        for c in range(6):
            k.tr(pA[:, c, :], qkr2[:, c * 128:(c + 1) * 128], identb[:], r=["qkr", "identb"], w=["ps5"])
        k.copy("vector", QAT[:, :, tok], pA[:, 0:4, :], r=["ps5"], w=["QAT"])
        k.copy("vector", KAT[:, :, tok], pA[:, 4:6, :], r=["ps5"], w=["KAT"])
        pB = PS[:, 6, :].bitcast(BF16).rearrange("p (c t) -> p c t", c=8)
        qkbr2 = qkbr.rearrange("p h d -> p (h d)")
        for c in range(8):
            k.tr(pB[:, c, :], qkbr2[:, c * 128:(c + 1) * 128], identb[:], r=["qkbr", "identb"], w=["ps6"])
        k.copy("scalar", stT[s_], pB, r=["ps6"], w=["stT"])
        k.dma("sync", QBs[:, tok].rearrange("(c p) t -> p c t", p=128), stT[s_][:, 0:4, :], r=["stT"])
        k.dma("sync", KBs[:, tok].rearrange("(c p) t -> p c t", p=128), stT[s_][:, 4:8, :], r=["stT"])
    k.barrier()

    def epilogue(src65, srckey, osb, rden, obf, slot, row0, col0, ncol):
        k.copy("vector", osb[0:65, 0:ncol], src65, r=[srckey], w=["osb%d" % slot])
        k.op("vector", lambda e: e.reciprocal(out=rden[64:65, 0:ncol], in_=osb[64:65, 0:ncol]), r=["osb%d" % slot], w=["rden%d" % slot])
        k.mm(PS[0:64, 7, 0:ncol], onesf[64:65, 0:64], rden[64:65, 0:ncol], True, True, r=["rden%d" % slot, "onesf"], w=["ps7"])
        k.tt("vector", obf[0:64, 0:ncol], osb[0:64, 0:ncol], PS[0:64, 7, 0:ncol], ALU.mult, r=["osb%d" % slot, "ps7"], w=["obf%d" % slot])
        k.dma("sync", OT[row0:row0 + 64, col0:col0 + ncol], obf[0:64, 0:ncol], r=["obf%d" % slot])

    ar.reset(base12)
    Pb = [ar.get([128, 512], BF16) for _ in range(3)]
    osb = [ar.get([128, 512], F32) for _ in range(2)]
    rden = [ar.get([128, 512], F32) for _ in range(2)]
    obf = [ar.get([128, 512], BF16) for _ in range(2)]
    VA4 = VA.rearrange("p n (k d) -> p n k d", k=2)
    Pb4 = [[Pb[0], Pb[1]], [Pb[2], ar.get([128, 512], BF16)]]
    for pair in range(4 if phases >= 2 else 0):
        kv = pair // 2
        for qb in range(16):
            qs = slice(qb * 512, (qb + 1) * 512)

            def qk_mm(kc):
                for hh in range(2):
                    b0 = 64 * hh
                    sb = 2 * hh + kc % 2
                    k.mm(PS[:, sb, :], KAT[b0:b0 + 64, kv, kc * 128:(kc + 1) * 128], QAT[b0:b0 + 64, pair, qs], True, True,
                         r=["QAT", "KAT"], w=["psS%d" % sb])
            qk_mm(0)
            for kc in range(NT):
                if kc + 1 < NT:
                    qk_mm(kc + 1)
                for hh in range(2):
                    sb = 2 * hh + kc % 2
                    k.act(Pb4[hh][kc % 2], PS[:, sb, :], AF.Exp, r=["psS%d" % sb], w=["Pb%d" % sb])
                for hh in range(2):
                    sb = 2 * hh + kc % 2
                    k.mm(PS[0:65, 4 + hh, :], VA4[:, kc, kv, :], Pb4[hh][kc % 2], kc == 0, kc == NT - 1, r=["VA", "Pb%d" % sb], w=["psO%d" % hh])
            for hh in range(2):
                epilogue(PS[0:65, 4 + hh, :], "psO%d" % hh, osb[hh], rden[hh], obf[hh], hh, (2 * pair + hh) * 64, qb * 512, 512)
    k.barrier()

    ar.reset(0)
    KBT = ar.get([128, S + 2048], BF16)
    QBT = ar.get([128, S], BF16)
    acc = ar.get([128, S], F32)
    vt = [ar.get([128, 65, 130], BF16) for _ in range(2)]
    Pe = [ar.get([128, 512], BF16) for _ in range(3)]
    Pm = [ar.get([128, 512], BF16) for _ in range(3)]
    osb3 = ar.get([128, 512], F32); rden3 = ar.get([128, 512], F32); obf3 = ar.get([128, 512], BF16)
    if phases >= 3:
        k.memset("gpsimd", KBT[:, 0:1024], 0.0, w=["KBT"])
        k.memset("gpsimd", KBT[:, S + 1024:S + 2048], 0.0, w=["KBT"])
    vi = 0
    gi = 0
    for pair in range(4 if phases >= 3 else 0):
        k.dma("sync", KBT[:, 1024:1024 + S], KBs[pair * 128:(pair + 1) * 128, :], w=["KBT"])
        k.dma("sync", QBT[:, :], QBs[pair * 128:(pair + 1) * 128, :], w=["QBT"])
        for hh in range(2):
            h = pair * 2 + hh
            b0 = 64 * hh
            k.memset("gpsimd", acc[0:65, :], 0.0, w=["acc"])
            for Dl in (1, 4, 16):
                L = S // Dl
                nq = L // 128
                for o in range(Dl):
                    vs = vi % 2
                    vi += 1
                    start = 1024 + o - 64 * Dl
                    nch = nq + 1
                    vsrc = VBs[start:start + Dl * (128 * nch - 1) + 1:Dl, pair * 130:(pair + 1) * 130].rearrange("(n p) c -> p n c", p=128)
                    k.dma("sync", vt[vs][:, 0:nch, :], vsrc, r=["VBs"], w=["vt%d" % vs])
                    for c0 in range(0, nq, 2):
                        g = gi % 3
                        gi += 1
                        psS = PS[:, g, :]
                        psO = PS[0:65, 3 + g, 0:256]
                        for cc in range(2):
                            c = c0 + cc
                            q0 = o + Dl * 128 * c
                            qsl = QBT[b0:b0 + 64, q0:q0 + Dl * 127 + 1:Dl]
                            for ty in range(2):
                                n = c + ty
                                k0 = 1024 + o + Dl * (128 * n - 64)
                                ksl = KBT[b0:b0 + 64, k0:k0 + Dl * 127 + 1:Dl]
                                j = cc * 2 + ty
                                k.mm(psS[:, j * 128:(j + 1) * 128], ksl, qsl, True, True, r=["KBT", "QBT"], w=["psS%d" % g])
                        k.act(Pe[g], psS, AF.Exp, r=["psS%d" % g], w=["Pe%d" % g])
                        k.tt("vector", Pm[g], Pe[g], maskAB[:], ALU.mult, r=["Pe%d" % g, "maskAB"], w=["Pm%d" % g])
                        for cc in range(2):
                            c = c0 + cc
                            for ty in range(2):
                                n = c + ty
                                j = cc * 2 + ty
                                k.mm(psO[:, cc * 128:(cc + 1) * 128], vt[vs][:, n, hh * 65:(hh + 1) * 65], Pm[g][:, j * 128:(j + 1) * 128],
                                     ty == 0, ty == 1, r=["vt%d" % vs, "Pm%d" % g], w=["psO%d" % g])
                        q0 = o + Dl * 128 * c0
                        av = acc[0:65, q0:q0 + Dl * 255 + 1:Dl]
                        k.tt("vector", av, psO, av, ALU.add, r=["psO%d" % g, "acc"], w=["acc"])
            for qb in range(16):
                epilogue(acc[0:65, qb * 512:(qb + 1) * 512], "acc", osb3, rden3, obf3, 0, 512 + h * 64, qb * 512, 512)
    k.barrier()

    ar.reset(0)
    wout = ar.get([128, 8, DM], BF16)
    logits = ar.get([128, NT, 36], F32)
    wth = ar.get([128, NT, NE], F32)
    mselp = ar.get([128, NT, NE], BF16)
    base4 = ar.off
    wst2 = ar.get([128, DM], F32)
    for jc in range(8 if phases >= 4 else 0):
        k.dma("sync", wst2, w_out_d[jc * 128:(jc + 1) * 128, :], w=["wst2"])
        k.ts("vector", wout[:, jc, :], wst2, gabc[:, jc:jc + 1], None, ALU.mult, r=["wst2", "gabc"], w=["wout"])
    ot = [ar.get([128, 8, 512], BF16) for _ in range(2)]
    osq = ar.get([128, 8, 128], F32)
    xt4 = ar.get([128, DM], F32)
    hbuf = [ar.get([128, DM], F32) for _ in range(2)]
    xn = ar.get([128, DM], F32)
    xnb = [ar.get([128, DM], BF16) for _ in range(2)]
    xnT = ar.get([128, 8, 128], F32)
    junk4 = ar.get([128, DM], BF16)
    st4 = ar.get([128, 16], F32)
    for t in range(NT if phases >= 4 else 0):
        s_ = t % 2
        tok = slice(t * 128, (t + 1) * 128)
        blk, sub = t // 4, t % 4
        bs = blk % 2
        if sub == 0:
            k.dma("sync", ot[bs], OT[:, blk * 512:(blk + 1) * 512].rearrange("(c p) t -> p c t", p=128), r=["OT"], w=["ot%d" % bs])
        k.dma("sync", xt4, x_d[tok, :], w=["xt4"])
        otl = ot[bs][:, :, sub * 128:(sub + 1) * 128]
        k.tt("gpsimd", osq, otl, otl, ALU.mult, r=["ot%d" % bs], w=["osq"])
        for half in range(2):
            for jc in range(4):
                k.mm(PS[:, 6, half:half + 1], osq[:, half * 4 + jc, :], onesf[:, 0:1], jc == 0, jc == 3, r=["osq", "onesf"], w=["ps6"])
        rstd(st4[:, 2:4], PS[:, 6, 0:2], 512, ["ps6"], ["rab"])
        for half in range(2):
            for part in range(2):
                for jc in range(4):
                    k.mm(PS[:, half * 2 + part, :], otl[:, part * 4 + jc, :], wout[:, part * 4 + jc, half * 512:(half + 1) * 512],
                         jc == 0, jc == 3, r=["ot%d" % bs, "wout"], w=["ps%d" % (half * 2 + part)])
        hb = hbuf[s_]
        for half in range(2):
            hs = slice(half * 512, (half + 1) * 512)
            k.stt("vector", hb[:, hs], PS[:, half * 2, :], st4[:, 2:3], xt4[:, hs], ALU.mult, ALU.add,
                  r=["ps%d" % (half * 2), "rab", "xt4"], w=["hb%d" % s_])
            k.stt("vector", hb[:, hs], PS[:, half * 2 + 1, :], st4[:, 3:4], hb[:, hs], ALU.mult, ALU.add,
                  r=["ps%d" % (half * 2 + 1), "rab", "hb%d" % s_], w=["hb%d" % s_])
        k.dma("sync", Hs[tok, :], hb, r=["hb%d" % s_])
        k.act(junk4, hb, AF.Square, r=["hb%d" % s_], w=["junk4", "ss2"], accum_out=st4[:, 4:5])
        rstd(st4[:, 5:6], st4[:, 4:5], DM, ["ss2"], ["r2"])
        k.stt("vector", xn, hb, st4[:, 5:6], g2bc[:], ALU.mult, ALU.mult, r=["hb%d" % s_, "r2", "g2bc"], w=["xn"])
        pT = PS[:, 4:6, :].rearrange("p a (c t) -> p (a c) t", c=4)
        for kc in range(8):
            k.tr(pT[:, kc, :], xn[:, kc * 128:(kc + 1) * 128], identf[:], r=["xn", "identf"], w=["ps45"])
        k.copy("scalar", xnT, pT, r=["ps45"], w=["xnT"])
        k.copy("gpsimd", xnb[s_], xn, r=["xn"], w=["xnb%d" % s_])
        k.dma("sync", XN[tok, :], xnb[s_], r=["xnb%d" % s_])
        for kc in range(8):
            k.mm(PS[:, 7, 0:36], xnT[:, kc, :], rwsb[:, kc, :], kc == 0, kc == 7, r=["xnT", "rwsb"], w=["ps7"])
        k.tt("vector", logits[:, t, :], PS[:, 7, 0:36], rbbc[:], ALU.add, r=["ps7", "rbbc"], w=["logits"])
    if debug and phases >= 4:
        k.dma("sync", LG[:, :], logits.rearrange("p t n -> p (t n)"), r=["logits"])
    k.barrier()

    ar.reset(base4)
    BIG = 1.0e30
    if phases >= 5:
        V = "vector"
        gl = logits[:, :, 0:4]
        el = logits[:, :, 4:36]
        gm = ar.get([128, NT], F32); ge = ar.get([128, NT, 4], F32); gs = ar.get([128, NT], F32)
        ohg = ar.get([128, NT, 4], F32); pen = ar.get([128, NT, 4], F32)
        em = ar.get([128, NT, 32], F32); em2 = ar.get([128, NT, 32], F32)
        m1 = ar.get([128, NT], F32); m2 = ar.get([128, NT], F32)
        oh1 = ar.get([128, NT, 32], F32); oh2 = ar.get([128, NT, 32], F32)
        p1 = ar.get([128, NT], F32); ga = ar.get([128, NT], F32); gb = ar.get([128, NT], F32)

        def b4(a):
            return a.unsqueeze(2).broadcast_to([128, NT, 4])

        def b32(a):
            return a.unsqueeze(2).broadcast_to([128, NT, 32])
        k.red(V, gm, gl, ALU.max, r=["logits"], w=["gm"])
        k.tt(V, ge, gl, b4(gm), ALU.subtract, r=["logits", "gm"], w=["ge"])
        k.tt(V, ohg, gl, b4(gm), ALU.is_equal, r=["logits", "gm"], w=["ohg"])
        k.act(ge, ge, AF.Exp, r=["ge"], w=["ge"])
        k.red(V, gs, ge, ALU.add, r=["ge"], w=["gs"])
        k.op(V, lambda e: e.reciprocal(out=gs, in_=gs), r=["gs"], w=["gs"])
        k.ts(V, pen, ohg, BIG, -BIG, ALU.mult, ALU.add, r=["ohg"], w=["pen"])
        k.tt(V, em.rearrange("p t (g e) -> p t g e", g=4), el.rearrange("p t (g e) -> p t g e", g=4),
             pen.unsqueeze(3).broadcast_to([128, NT, 4, 8]), ALU.add, r=["logits", "pen"], w=["em"])
        k.red(V, m1, em, ALU.max, r=["em"], w=["m1"])
        k.tt(V, oh1, em, b32(m1), ALU.is_equal, r=["em", "m1"], w=["oh1"])
        k.stt(V, em2, oh1, -BIG, em, ALU.mult, ALU.add, r=["oh1", "em"], w=["em2"])
        k.red(V, m2, em2, ALU.max, r=["em2"], w=["m2"])
        k.tt(V, oh2, em2, b32(m2), ALU.is_equal, r=["em2", "m2"], w=["oh2"])
        k.tt(V, p1, m2, m1, ALU.subtract, r=["m1", "m2"], w=["p1"])
        k.act(p1, p1, AF.Exp, r=["p1"], w=["p1"])
        k.ts(V, p1, p1, 1.0, None, ALU.add, r=["p1"], w=["p1"])
        k.op(V, lambda e: e.reciprocal(out=p1, in_=p1), r=["p1"], w=["p1"])
        k.tt(V, mselp, oh1, oh2, ALU.add, r=["oh1", "oh2"], w=["mselp"])
        msel2 = mselp.rearrange("p t e -> p (t e)")
        wth2 = wth.rearrange("p t e -> p (t e)")
        for q in range(4):
            k.mm(PS[:, q, :], triU[:], msel2[:, q * 512:(q + 1) * 512], True, True, r=["mselp", "triU"], w=["ps%d" % q])
            k.copy("scalar", wth2[:, q * 512:(q + 1) * 512], PS[:, q, :], r=["ps%d" % q], w=["wth"])
        k.tt(V, ga, gs, p1, ALU.mult, r=["gs", "p1"], w=["ga"])
        k.tt(V, gb, gs, ga, ALU.subtract, r=["gs", "ga"], w=["gb"])
        k.tt(V, oh1, oh1, b32(ga), ALU.mult, r=["oh1", "ga"], w=["oh1"])
        k.tt(V, oh2, oh2, b32(gb), ALU.mult, r=["oh2", "gb"], w=["oh2"])
        k.tt(V, Gfull, oh1, oh2, ALU.add, r=["oh1", "oh2"], w=["Gfull"])
        if debug:
            k.dma("sync", DST[:, :], Gfull.rearrange("p t e -> p (t e)"), r=["Gfull"])
    k.barrier()

    EG = [(g * 4, 4) for g in range(8)]
    V = "vector"

    def build_sel(c, selc, gsc=None, pre=""):
        w3 = wth[:, c, :].unsqueeze(2).broadcast_to([128, NE, CT])
        io = iota24[:, :].unsqueeze(1).broadcast_to([128, NE, CT])
        k.tt("vector", selc, w3, io, ALU.is_equal, r=["wth", "iota24"], w=[pre + "selc"])
        k.tt("gpsimd", selc, selc, mselp[:, c, :].unsqueeze(2).broadcast_to([128, NE, CT]), ALU.mult, r=[pre + "selc", "mselp"], w=[pre + "selc"])
        if gsc is not None:
            k.tt("gpsimd", gsc, selc, Gfull[:, c, :].unsqueeze(2).broadcast_to([128, NE, CT]), ALU.mult, r=[pre + "selc", "Gfull"], w=[pre + "gsc"])

    if phases >= 5:
        ar.reset(base4)
        xg = [ar.get([128, DM], BF16) for _ in range(2)]
        selc_ = [ar.get([128, NE, CT], BF16) for _ in range(2)]
        xrow = [ar.get([128, DM], BF16) for _ in range(4)]
        xi = 0
        k.dma("sync", xg[0], XN[0:128, :], r=["XN"], w=["xg0"])
        for c in range(NT):
            s_ = c % 2
            if c + 1 < NT:
                k.dma("sync", xg[1 - s_], XN[(c + 1) * 128:(c + 2) * 128, :], r=["XN"], w=["xg%d" % (1 - s_)])
            build_sel(c, selc_[s_], None, "d%d" % s_)
            sel2 = selc_[s_].rearrange("p e j -> p (e j)")
            for (e0, ne) in EG:
                M = ne * CT
                xr = xi % 4
                xi += 1
                for half in range(2):
                    bk = (xr % 2) * 2 + half
                    k.mm(PS[0:M, bk, :], sel2[:, e0 * CT:e0 * CT + M], xg[s_][:, half * 512:(half + 1) * 512], True, True,
                         r=["d%dselc" % s_, "xg%d" % s_], w=["psd%d" % bk])
                    k.copy("scalar" if half == 0 else "vector", xrow[xr][0:M, half * 512:(half + 1) * 512], PS[0:M, bk, :],
                           r=["psd%d" % bk], w=["xrow%d" % xr])
                for el in range(ne):
                    k.dma("sync", XS[e0 + el, c * CT:(c + 1) * CT, :], xrow[xr][el * CT:(el + 1) * CT, :], r=["xrow%d" % xr])
        k.barrier()

        ar.reset(base4)
        wg = [ar.get([128, 8, 512], BF16) for _ in range(2)]
        wu = [ar.get([128, 8, 512], BF16) for _ in range(2)]
        wd = [ar.get([128, 4, DM], BF16) for _ in range(2)]
        xs = [ar.get([128, 4, DM], BF16) for _ in range(2)]
        xsT = ar.get([128, 8, 512], BF16)
        sg = [ar.get([128, 512], F32) for _ in range(2)]
        hT = ar.get([128, 4, 512], BF16)
        ysb = [ar.get([128, DM], F32) for _ in range(2)]
        yi = 0
        NBLK = RE // 512
        blocks = [(e_, b_) for e_ in range(NE) for b_ in range(NBLK)]

        def load_w(e_):
            ws = e_ % 2
            k.dma("gpsimd", wg[ws], wg_d[e_].rearrange("(k p) f -> p k f", p=128), w=["wg%d" % ws])
            k.dma("gpsimd", wu[ws], wu_d[e_].rearrange("(k p) f -> p k f", p=128), w=["wu%d" % ws])
            k.dma("gpsimd", wd[ws], wd_d[e_].rearrange("(k p) f -> p k f", p=128), w=["wd%d" % ws])

        def load_x(i):
            e_, b_ = blocks[i]
            k.dma("sync", xs[i % 2], XS[e_, b_ * 512:(b_ + 1) * 512, :].rearrange("(a p) d -> p a d", p=128), r=["XS"], w=["xs%d" % (i % 2)])
        load_w(0)
        load_x(0)
        for i, (e_, blk) in enumerate(blocks):
            ws = e_ % 2
            xb = i % 2
            r0 = blk * 512
            if blk == 0 and e_ + 1 < NE:
                load_w(e_ + 1)
            if i + 1 < len(blocks):
                load_x(i + 1)
            for kc in range(8):
                tb = kc % 2
                pT = PS[:, tb, 0:256].bitcast(BF16).rearrange("p (a t) -> p a t", a=4)
                for a in range(4):
                    k.tr(pT[:, a, :], xs[xb][:, a, kc * 128:(kc + 1) * 128], identb[:], r=["xs%d" % xb, "identb"], w=["pst%d" % tb])
                k.copy("vector" if kc % 2 == 0 else "scalar", xsT[:, kc, :], PS[:, tb, 0:256].bitcast(BF16), r=["pst%d" % tb], w=["xsT"])
            for fc in range(4):
                pb = fc % 2
                for kc in range(8):
                    k.mm(PS[:, 2 + pb, :], wg[ws][:, kc, fc * 128:(fc + 1) * 128], xsT[:, kc, :], kc == 0, kc == 7,
                         r=["wg%d" % ws, "xsT"], w=["psg%d" % pb])
                for kc in range(8):
                    k.mm(PS[:, 4 + pb, :], wu[ws][:, kc, fc * 128:(fc + 1) * 128], xsT[:, kc, :], kc == 0, kc == 7,
                         r=["wu%d" % ws, "xsT"], w=["psu%d" % pb])
                k.act(sg[pb], PS[:, 2 + pb, :], AF.Silu, r=["psg%d" % pb], w=["sg%d" % pb])
                k.tt("vector", hT[:, fc, :], sg[pb], PS[:, 4 + pb, :], ALU.mult, r=["sg%d" % pb, "psu%d" % pb], w=["hT"])
            for rt in range(4):
                ys = yi % 2
                yi += 1
                for dh in range(2):
                    for fc in range(4):
                        k.mm(PS[:, 6 + dh, :], hT[:, fc, rt * 128:(rt + 1) * 128], wd[ws][:, fc, dh * 512:(dh + 1) * 512], fc == 0, fc == 3,
                             r=["hT", "wd%d" % ws], w=["psy%d" % dh])
                k.copy("scalar", ysb[ys][:, 0:512], PS[:, 6, :], r=["psy0"], w=["ysb%d" % ys])
                k.copy("vector", ysb[ys][:, 512:1024], PS[:, 7, :], r=["psy1"], w=["ysb%d" % ys])
                k.dma("sync", Ys[e_, r0 + rt * 128:r0 + (rt + 1) * 128, :], ysb[ys], r=["ysb%d" % ys])
        k.barrier()

        ar.reset(base4)
        selc6 = [ar.get([128, NE, CT], BF16) for _ in range(2)]
        gsc6 = [ar.get([128, NE, CT], F32) for _ in range(2)]
        gt = [ar.get([128, len(EG), 128], F32) for _ in range(2)]
        yr = [ar.get([128, len(EG), DM], F32) for _ in range(2)]
        hh_ = [ar.get([128, DM], F32) for _ in range(2)]
        ob = [ar.get([128, DM], F32) for _ in range(2)]
        junk6 = ar.get([128, DM], BF16)
        st6 = ar.get([128, 2 * NT], F32)
        def load6(c):
            s_ = c % 2
            k.dma("sync", hh_[s_], Hs[c * 128:(c + 1) * 128, :], r=["Hs"], w=["hh%d" % s_])
            for gi_, (e0, ne) in enumerate(EG):
                for el in range(ne):
                    k.dma("sync", yr[s_][el * CT:(el + 1) * CT, gi_, :], Ys[e0 + el, c * CT:(c + 1) * CT, :], r=["Ys"], w=["yr%d" % s_])
        def prep6(c):
            s_ = c % 2
            build_sel(c, selc6[s_], gsc6[s_], "c%d" % s_)
            gs2 = gsc6[s_].rearrange("p e j -> p (e j)")
            for gi_, (e0, ne) in enumerate(EG):
                M = ne * CT
                bk = gi_ % 2
                k.tr(PS[0:M, bk, 0:128], gs2[:, e0 * CT:e0 * CT + M], identf[:], r=["c%dgsc" % s_, "identf"], w=["pst%d" % bk])
                k.copy("scalar", gt[s_][0:M, gi_, :], PS[0:M, bk, 0:128], r=["pst%d" % bk], w=["gt%d" % s_])
        load6(0)
        prep6(0)
        for c in range(NT):
            s_ = c % 2
            tok = slice(c * 128, (c + 1) * 128)
            if c + 1 < NT:
                load6(c + 1)
            for half in range(2):
                for gi_, (e0, ne) in enumerate(EG):
                    M = ne * CT
                    k.mm(PS[:, 2 + half, :], gt[s_][0:M, gi_, :], yr[s_][0:M, gi_, half * 512:(half + 1) * 512], gi_ == 0, gi_ == len(EG) - 1,
                         r=["gt%d" % s_, "yr%d" % s_], w=["psc%d" % half])
            if c + 1 < NT:
                prep6(c + 1)
            for half in range(2):
                hs = slice(half * 512, (half + 1) * 512)
                k.tt("vector", hh_[s_][:, hs], hh_[s_][:, hs], PS[:, 2 + half, :], ALU.add, r=["psc%d" % half, "hh%d" % s_], w=["hh%d" % s_])
            k.act(junk6, hh_[s_], AF.Square, r=["hh%d" % s_], w=["junk6", "ss6_%d" % s_], accum_out=st6[:, 2 * c:2 * c + 1])
            rstd(st6[:, 2 * c + 1:2 * c + 2], st6[:, 2 * c:2 * c + 1], DM, ["ss6_%d" % s_], ["r6_%d" % s_])
            k.stt("vector", ob[s_], hh_[s_], st6[:, 2 * c + 1:2 * c + 2], gfbc[:], ALU.mult, ALU.mult,
                  r=["hh%d" % s_, "r6_%d" % s_, "gfbc"], w=["ob%d" % s_])
            k.dma("sync", out_d[tok, :], ob[s_], r=["ob%d" % s_])
    k.barrier()

    sems = {}
    pools = {}
    import contextlib
    with contextlib.ExitStack() as es:
        for e in ENGS:
            sems[e] = es.enter_context(nc.semaphore("s_" + e))
            pools[e] = [es.enter_context(nc.semaphore("d_%s%d" % (e, i))) for i in range(NPOOL)]
        k.emit(sems, pools)
    return nc


def _consts():
    ident = np.eye(128, dtype=np.float32)
    i = np.arange(128)[:, None]; j = np.arange(128)[None, :]
    triu = (i < j).astype(np.float32)
    mA = (j <= i).astype(np.float32)
    mB = (j >= i).astype(np.float32)
    mask = np.concatenate([mA, mB, mA, mB], axis=1)
    return ident, triu, mask


def _tables():
    def inv_freq(dim):
        return (1.0 / (10000.0 ** (np.arange(0, dim, 2, dtype=np.float32) / np.float32(dim)))).astype(np.float32)
    t = np.arange(S, dtype=np.float32)
    row = np.floor(t / 64).astype(np.float32); col = (t % 64).astype(np.float32)
    f = inv_freq(32)
    angA = np.concatenate([row[:, None] * f, col[:, None] * f], axis=-1).astype(np.float32)
    angB = (t[:, None] * inv_freq(64)).astype(np.float32)
    return np.concatenate([np.cos(angA), np.sin(angA), np.cos(angB), np.sin(angB)], axis=-1).astype(np.float32)


def make_in_maps(inp, cores):
    ident, triu, mask = _consts()
    tabs = _tables()
    f = lambda a: np.ascontiguousarray(a, dtype=np.float32)
    shared = {
        "w_in": f(inp["w_in"][0]), "w_out": f(inp["w_out"][0]),
        "g1": f(inp["norm1_g"][0].reshape(8, 128).T),
        "gab": f(np.concatenate([inp["out_norm_a_g"][0], inp["out_norm_b_g"][0]]).reshape(8, 128).T),
        "g2": f(inp["norm2_g"][0]), "gf": f(inp["final_norm_g"]),
        "gq": f(inp["q_norm_g"][0]), "gk": f(inp["k_norm_g"][0]),
        "rw": f(np.concatenate([inp["router_group_w"][0], inp["router_expert_w"][0]], axis=1)),
        "rb": f(np.concatenate([inp["router_group_b"][0], inp["router_expert_b"][0]])),
        "w_gate": f(inp["w_gate"][0]), "w_up": f(inp["w_up"][0]), "w_down": f(inp["w_down"][0]),
        "tabs": tabs, "c_ident": ident, "c_triu": triu, "c_mask": mask,
        "c_e1024": (np.arange(NE) * CAP).astype(np.float32),
        "c_iota": np.arange(CT).astype(np.float32),
    }
    maps = []
    for c in cores:
        m = dict(shared)
        xb = f(inp["x"][c])
        m["x"] = xb
        maps.append(m)
    return maps


def kernel(**inputs):
    inp = {k_: np.asarray(v) for k_, v in inputs.items()}
    nc = build_nc(debug=False)
    maps = make_in_maps(inp, list(range(8)))
    res = run_bass_kernel_spmd(nc, maps, core_ids=list(range(8)))
    out = np.stack([np.asarray(r["out"]) for r in res.results], axis=0)
    return out.astype(np.float32)
```

```python
import numpy as np
import concourse.bass as bass
import concourse.mybir as mybir
from concourse.bass_utils import run_bass_kernel_spmd

F32 = mybir.dt.float32
BF16 = mybir.dt.bfloat16
I32 = mybir.dt.int32
ALU = mybir.AluOpType
AF = mybir.ActivationFunctionType
AX = mybir.AxisListType

S = 8192
NT = 64
DM = 1024
CAP = 1024
CT = 32
RE = NT * CT
NE = 32
EPS = 1e-6
ENGS = ["tensor", "vector", "scalar", "gpsimd", "sync"]
NPOOL = 16
DEBUG = False


class KB:
    def __init__(self, nc):
        self.nc = nc
        self.ops = {e: [] for e in ENGS}
        self.last_w = {}
        self.readers = {}
        self.ndma = {e: 0 for e in ENGS}
        self.dmas = {e: [] for e in ENGS}

    def op(self, eng, fn, r=(), w=(), dma=False):
        rec = dict(eng=eng, fn=fn, deps=[], sig=False, dma=dma, idx=len(self.ops[eng]))
        deps = []
        for k in r:
            if k in self.last_w:
                deps.append(self.last_w[k])
        for k in w:
            if k in self.last_w:
                deps.append(self.last_w[k])
            deps.extend(self.readers.get(k, ()))
        for k in r:
            self.readers.setdefault(k, []).append(rec)
        for k in w:
            self.last_w[k] = rec
            self.readers[k] = []
        if dma:
            rec["k"] = self.ndma[eng]
            self.ndma[eng] += 1
            self.dmas[eng].append(rec)
        seen = set()
        for d in deps:
            if id(d) in seen or d is rec:
                continue
            seen.add(id(d))
            if d["eng"] == "tensor" and eng == "tensor" and not d["dma"]:
                continue
            d["sig"] = True
            rec["deps"].append(d)
        self.ops[eng].append(rec)
        return rec

    def barrier(self):
        lasts = []
        for e in ENGS:
            for rec in reversed(self.ops[e]):
                if rec["fn"] is not None and not rec["dma"]:
                    rec["sig"] = True
                    lasts.append(rec)
                    break
            lasts.extend(self.dmas[e][-NPOOL:])
        for e in ENGS:
            rec = dict(eng=e, fn=None, deps=list(lasts), sig=False, dma=False, idx=len(self.ops[e]))
            self.ops[e].append(rec)
        self.last_w = {}
        self.readers = {}

    def emit(self, sems, pools):
        nc = self.nc
        for e in ENGS:
            c = 0
            for rec in self.ops[e]:
                if rec["dma"]:
                    k = rec["k"]
                    rec["sem"] = pools[e][k % NPOOL]
                    rec["val"] = 16 * (k // NPOOL + 1)
                elif rec["sig"]:
                    c += 1
                    rec["sem"] = sems[e]
                    rec["val"] = c
        with nc.Block() as block:
            for e in ENGS:
                def mk(e):
                    def body(eng):
                        seen = {}
                        def wait(sem, val):
                            key = id(sem)
                            if seen.get(key, 0) >= val:
                                return
                            seen[key] = val
                            eng.wait_ge(sem, val)
                        for rec in self.ops[e]:
                            for d in rec["deps"]:
                                wait(d["sem"], d["val"])
                            if rec["fn"] is None:
                                continue
                            if rec["dma"]:
                                k = rec["k"]
                                if k >= NPOOL:
                                    wait(pools[e][k % NPOOL], 16 * (k // NPOOL))
                                rec["fn"](eng).then_inc(rec["sem"], 16)
                            else:
                                ins = rec["fn"](eng)
                                if rec["sig"]:
                                    ins.then_inc(rec["sem"], 1)
                    return body
                getattr(block, e)(mk(e))

    def mm(self, out, lhsT, rhs, start, stop, r=(), w=()):
        return self.op("tensor", lambda e: e.matmul(out, lhsT=lhsT, rhs=rhs, start=start, stop=stop), r, w)

    def tr(self, out, in_, ident, r=(), w=()):
        return self.op("tensor", lambda e: e.transpose(out, in_, ident), r, w)

    def act(self, out, in_, func, r=(), w=(), **kw):
        return self.op("scalar", lambda e: e.activation(out=out, in_=in_, func=func, **kw), r, w)

    def tt(self, eng, out, in0, in1, op, r=(), w=()):
        return self.op(eng, lambda e: e.tensor_tensor(out=out, in0=in0, in1=in1, op=op), r, w)

    def ts(self, eng, out, in0, s1, s2, op0, op1=None, r=(), w=()):
        if op1 is None:
            return self.op(eng, lambda e: e.tensor_scalar(out=out, in0=in0, scalar1=s1, scalar2=None, op0=op0), r, w)
        return self.op(eng, lambda e: e.tensor_scalar(out=out, in0=in0, scalar1=s1, scalar2=s2, op0=op0, op1=op1), r, w)

    def stt(self, eng, out, in0, scalar, in1, op0, op1, r=(), w=()):
        return self.op(eng, lambda e: e.scalar_tensor_tensor(out=out, in0=in0, scalar=scalar, in1=in1, op0=op0, op1=op1), r, w)

    def copy(self, eng, out, in_, r=(), w=()):
        if eng == "scalar":
            return self.op(eng, lambda e: e.activation(out=out, in_=in_, func=AF.Copy), r, w)
        return self.op(eng, lambda e: e.tensor_copy(out=out, in_=in_), r, w)

    def red(self, eng, out, in_, op, r=(), w=()):
        return self.op(eng, lambda e: e.tensor_reduce(out=out, in_=in_, axis=AX.X, op=op), r, w)

    def memset(self, eng, ap, val, r=(), w=()):
        return self.op(eng, lambda e: e.memset(ap, val), r, w)

    def dma(self, q, out, in_, r=(), w=()):
        return self.op(q, lambda e: e.dma_start(out=out, in_=in_), r, w, dma=True)

    def scatter(self, out, off, in_, bound, r=(), w=()):
        return self.op("gpsimd", lambda e: e.indirect_dma_start(
            out=out, out_offset=bass.IndirectOffsetOnAxis(ap=off, axis=0), in_=in_, in_offset=None,
            bounds_check=bound, oob_is_err=False), r, w, dma=True)

    def gather(self, out, in_, off, bound, r=(), w=()):
        return self.op("gpsimd", lambda e: e.indirect_dma_start(
            out=out, out_offset=None, in_=in_, in_offset=bass.IndirectOffsetOnAxis(ap=off, axis=0),
            bounds_check=bound, oob_is_err=False), r, w, dma=True)


def build_nc(debug=False, phases=6):
    nc = bass.Bass("TRN2", target_bir_lowering=False)
    dk = "ExternalOutput" if debug else "Internal"

    def din(name, shape, dt=F32):
        return nc.dram_tensor(name, list(shape), dt, kind="ExternalInput")

    x_d = din("x", [S, DM])
    w_in_d = din("w_in", [DM, 2304]); w_out_d = din("w_out", [DM, DM])
    g1_d = din("g1", [128, 8]); gab_d = din("gab", [128, 8]); g2_d = din("g2", [DM]); gf_d = din("gf", [DM])
    gq_d = din("gq", [64]); gk_d = din("gk", [64])
    rw_d = din("rw", [DM, 36]); rb_d = din("rb", [36])
    wg_d = din("w_gate", [NE, DM, 512]); wu_d = din("w_up", [NE, DM, 512]); wd_d = din("w_down", [NE, 512, DM])
    tabs_d = din("tabs", [S, 128])
    cid_d = din("c_ident", [128, 128]); ctri_d = din("c_triu", [128, 128]); cmask_d = din("c_mask", [128, 512])
    ce_d = din("c_e1024", [NE])
    cio_d = din("c_iota", [CT])
    out_d = nc.dram_tensor("out", [S, DM], F32, kind="ExternalOutput")

    QBs = nc.dram_tensor("QBs", [512, S], BF16, kind="Internal")
    KBs = nc.dram_tensor("KBs", [512, S], BF16, kind="Internal")
    VBs = nc.dram_tensor("VBs", [S + 2048, 520], BF16, kind="Internal")
    OT = nc.dram_tensor("OT", [DM, S], BF16, kind=dk)
    Hs = nc.dram_tensor("Hs", [S, DM], F32, kind=dk)
    XN = nc.dram_tensor("XN", [S, DM], BF16, kind="Internal")
    XS = nc.dram_tensor("XS", [NE, RE, DM], BF16, kind="Internal")
    Ys = nc.dram_tensor("Ys", [NE, RE, DM], BF16, kind="Internal")
    LG = nc.dram_tensor("LG", [128, NT * 36], F32, kind=dk)
    DST = nc.dram_tensor("DST", [128, NT * NE], F32, kind=dk)

    def A(name, shape, dt):
        t_ = nc.alloc_sbuf_tensor(name, shape, dt)
        return t_[tuple(slice(None) for _ in shape)]
    identb = A("identb", [128, 128], BF16); identf = A("identf", [128, 128], F32)
    maskAB = A("maskAB", [128, 512], BF16); triU = A("triU", [128, 128], BF16)
    onesb = A("onesb", [128, 128], BF16); onesf = A("onesf", [128, 64], F32)
    g1c = A("g1c", [128, 8], F32); gabc = A("gabc", [128, 8], F32)
    gqk = A("gqk", [128, 12, 64], F32)
    g2bc = A("g2bc", [128, DM], F32); gfbc = A("gfbc", [128, DM], F32)
    rwsb = A("rwsb", [128, 8, 36], F32); rbbc = A("rbbc", [128, 36], F32)
    e1024 = A("e1024", [128, NE], F32)
    iota24 = A("iota24", [128, CT], F32)
    Gfull = A("Gfull", [128, NT, NE], F32)
    ARN = 94400
    arena = A("arena", [128, ARN], BF16)
    PS = nc.alloc_psum_tensor("PS", [128, 8, 512], F32)

    class Arena:
        def __init__(self):
            self.off = 0

        def reset(self, off=0):
            self.off = off

        def get(self, shape, dt):
            n = int(np.prod(shape[1:]))
            nb = n * (2 if dt == BF16 else 4)
            nb = (nb + 63) // 64 * 64
            a = self.off // 2
            self.off += nb
            assert self.off <= ARN * 2, ("arena overflow", self.off)
            v = arena[:, a:a + (n if dt == BF16 else 2 * n)]
            if dt != BF16:
                v = v.bitcast(dt)
            if len(shape) == 3:
                v = v.rearrange("p (a b) -> p a b", a=shape[1])
            elif len(shape) == 4:
                v = v.rearrange("p (a b c) -> p a b c", a=shape[1], b=shape[2])
            return v

    ar = Arena()
    k = KB(nc)
    epsc = A("epsc", [128, 1], F32)
    k.memset("vector", epsc[:], EPS, w=["epsc"])

    def rstd(out, in_, n, rk, wk):
        k.act(out, in_, AF.Sqrt, r=rk + ["epsc"], w=wk, scale=1.0 / n, bias=epsc[:, 0:1])
        k.op("vector", lambda e: e.reciprocal(out=out, in_=out), r=wk, w=wk)

    k.dma("gpsimd", identb[:], cid_d[:, :], w=["identb"])
    k.dma("sync", identf[:], cid_d[:, :], w=["identf"])
    k.dma("gpsimd", maskAB[:], cmask_d[:, :], w=["maskAB"])
    k.dma("gpsimd", triU[:], ctri_d[:, :], w=["triU"])
    k.memset("vector", onesb[:], 1.0, w=["onesb"])
    k.memset("vector", onesf[:], 1.0, w=["onesf"])
    k.dma("sync", g1c[:], g1_d[:, :], w=["g1c"])
    k.dma("sync", gabc[:], gab_d[:, :], w=["gabc"])
    for h in range(12):
        src = gq_d if h < 8 else gk_d
        k.dma("sync", gqk[:, h, :], src[:].partition_broadcast(128), w=["gqk"])
    k.ts("vector", gqk[:, 0:8, :], gqk[:, 0:8, :], 0.125, None, ALU.mult, r=["gqk"], w=["gqk"])
    k.dma("sync", g2bc[:], g2_d[:].partition_broadcast(128), w=["g2bc"])
    k.dma("sync", gfbc[:], gf_d[:].partition_broadcast(128), w=["gfbc"])
    k.dma("sync", rwsb[:], rw_d[:, :].rearrange("(k p) n -> p k n", p=128), w=["rwsb"])
    k.dma("sync", rbbc[:], rb_d[:].partition_broadcast(128), w=["rbbc"])
    k.dma("sync", e1024[:], ce_d[:].partition_broadcast(128), w=["e1024"])
    k.dma("sync", iota24[:], cio_d[:].partition_broadcast(128), w=["iota24"])

    win = ar.get([128, 8, 2432], BF16)
    QAT = ar.get([128, 4, S], BF16)
    KAT = ar.get([128, 2, S], BF16)
    VA = ar.get([128, NT, 130], BF16)
    base12 = ar.off

    wst = ar.get([128, 2304], F32)
    zt = ar.get([128, 8, 520], BF16)
    for kc in range(8):
        k.dma("sync", wst, w_in_d[kc * 128:(kc + 1) * 128, :], w=["wst"])
        gcol = g1c[:, kc:kc + 1]
        k.ts("vector", win[:, kc, 0:512], wst[:, 0:512], gcol, None, ALU.mult, r=["wst", "g1c"], w=["win"])
        k.ts("vector", win[:, kc, 512:2048], wst[:, 768:2304], gcol, None, ALU.mult, r=["wst", "g1c"], w=["win"])
        k.ts("gpsimd", win[:, kc, 2048:2304].rearrange("p (a b c) -> p a b c", a=2, b=2),
             wst[:, 512:640].rearrange("p (a b c) -> p a b c", a=2, b=1).broadcast_to([128, 2, 2, 64]),
             gcol, None, ALU.mult, r=["wst", "g1c"], w=["win"])
        k.ts("gpsimd", win[:, kc, 2304:2432], wst[:, 640:768], gcol, None, ALU.mult, r=["wst", "g1c"], w=["win"])
    k.memset("gpsimd", zt, 0.0, w=["zt"])
    k.dma("sync", VBs[0:1024, :].rearrange("(n p) c -> p n c", p=128), zt, r=["zt"])
    k.dma("sync", VBs[S + 1024:S + 2048, :].rearrange("(n p) c -> p n c", p=128), zt, r=["zt"])
    k.memset("gpsimd", VA.rearrange("p n (k d) -> p n k d", k=2)[:, :, :, 64:65], 1.0, w=["VA"])
    k.barrier()

    ar.reset(base12)
    xTb_ = ar.get([128, 8, 128], BF16); xTb = [xTb_, xTb_]
    xt = ar.get([128, DM], F32)
    junk = ar.get([128, DM], BF16)
    tab = [ar.get([128, 128], F32) for _ in range(2)]
    qk = ar.get([128, 12, 64], F32)
    sq = ar.get([128, 12, 64], F32)
    qkr = ar.get([128, 12, 64], BF16)
    t1 = ar.get([128, 12, 32], F32); t2 = ar.get([128, 12, 32], F32)
    qkb = ar.get([128, 16, 64], F32)
    qkbr = ar.get([128, 16, 64], BF16)
    u1 = ar.get([128, 16, 32], F32); u2 = ar.get([128, 16, 32], F32)
    vbst = [ar.get([128, 8, 65], BF16) for _ in range(2)]
    stT_ = ar.get([128, 8, 128], BF16); stT = [stT_, stT_]
    st1 = ar.get([128, 32], F32)
    for s_ in range(2):
        k.memset("gpsimd", vbst[s_][:, :, 64:65], 1.0, w=["vbst%d" % s_])

    def rotary(eng, src, dst, ta, tb, cs, sn, nh, kr, kw, pre):
        C = cs.unsqueeze(1).broadcast_to([128, nh, 32]); Sn = sn.unsqueeze(1).broadcast_to([128, nh, 32])
        x1 = src[:, :, 0:32]; x2 = src[:, :, 32:64]
        k.tt(eng, ta, x1, C, ALU.mult, r=kr, w=[pre + "a"])
        k.tt(eng, tb, x2, Sn, ALU.mult, r=kr, w=[pre + "b"])
        k.tt(eng, dst[:, :, 0:32], ta, tb, ALU.subtract, r=[pre + "a", pre + "b"], w=kw)
        k.tt(eng, ta, x2, C, ALU.mult, r=kr + kw, w=[pre + "a"])
        k.tt(eng, tb, x1, Sn, ALU.mult, r=kr + kw, w=[pre + "b"])
        k.tt(eng, dst[:, :, 32:64], ta, tb, ALU.add, r=[pre + "a", pre + "b"], w=kw)

    for t in range(NT if phases >= 1 else 0):
        s_ = t % 2
        tok = slice(t * 128, (t + 1) * 128)
        k.dma("sync", xt, x_d[tok, :], w=["xt"])
        k.dma("sync", tab[s_], tabs_d[tok, :], w=["tab%d" % s_])
        sqj = sq.rearrange("p h d -> p (h d)").bitcast(BF16)[:, 0:DM]
        k.act(sqj, xt, AF.Square, r=["xt"], w=["sq", "ss"], accum_out=st1[:, 0:1])
        k.copy("gpsimd", junk, xt, r=["xt"], w=["junk"])
        pX = PS[:, 7, :].bitcast(BF16).rearrange("p (c t) -> p c t", c=8)
        for kc in range(8):
            k.tr(pX[:, kc, :], junk[:, kc * 128:(kc + 1) * 128], identb[:], r=["junk", "identb"], w=["ps7"])
        k.copy("vector", xTb[s_], pX, r=["ps7"], w=["xTb"])
        rstd(st1[:, 1:2], st1[:, 0:1], DM, ["ss"], ["rstd1"])
        for b in range(5):
            n0, n1 = b * 512, min((b + 1) * 512, 2432)
            for kc in range(8):
                k.mm(PS[:, b, 0:n1 - n0], xTb[s_][:, kc, :], win[:, kc, n0:n1], kc == 0, kc == 7,
                     r=["xTb", "win"], w=["ps%d" % b])
        rs = st1[:, 1:2]
        k.act(qk[:, 0:8, :], PS[:, 0, :].rearrange("p (h d) -> p h d", h=8), AF.Copy, r=["ps0", "rstd1"], w=["qk"], scale=rs)
        k.act(qk[:, 8:12, :], PS[:, 4, 0:256].rearrange("p (h d) -> p h d", h=4), AF.Copy, r=["ps4", "rstd1"], w=["qk"], scale=rs)
        k.act(VA[:, t, :].rearrange("p (k d) -> p k d", k=2)[:, :, 0:64], PS[:, 4, 256:384].rearrange("p (k d) -> p k d", k=2),
              AF.Copy, r=["ps4", "rstd1"], w=["VA"], scale=rs)
        k.tt("vector", sq, qk, qk, ALU.mult, r=["qk"], w=["sq"])
        k.red("vector", st1[:, 2:14], sq, ALU.add, r=["sq"], w=["ssq"])
        rstd(st1[:, 14:26], st1[:, 2:14], 64, ["ssq"], ["rq"])
        k.tt("vector", qk, qk, st1[:, 14:26].unsqueeze(2).broadcast_to([128, 12, 64]), ALU.mult, r=["qk", "rq"], w=["qk"])
        k.tt("vector", qk, qk, gqk, ALU.mult, r=["qk", "gqk"], w=["qk"])
        rotary("vector", qk, qkr, t1, t2, tab[s_][:, 0:32], tab[s_][:, 32:64], 12, ["qk", "tab%d" % s_], ["qkr"], "t")
        k.act(qkb[:, 0:8, :], PS[:, 1, :].rearrange("p (h d) -> p h d", h=8), AF.Copy, r=["ps1", "rstd1"], w=["qkb"], scale=rs)
        k.act(qkb[:, 8:16, :], PS[:, 2, :].rearrange("p (h d) -> p h d", h=8), AF.Copy, r=["ps2", "rstd1"], w=["qkb"], scale=rs)
        k.ts("gpsimd", qkb[:, 0:8, :], qkb[:, 0:8, :], 0.125, None, ALU.mult, r=["qkb"], w=["qkb"])
        rotary("gpsimd", qkb, qkbr, u1, u2, tab[s_][:, 64:96], tab[s_][:, 96:128], 16, ["qkb", "tab%d" % s_], ["qkbr"], "u")
        k.act(vbst[s_][:, :, 0:64], PS[:, 3, :].rearrange("p (h d) -> p h d", h=8), AF.Copy,
              r=["ps3", "rstd1"], w=["vbst%d" % s_], scale=rs)
        k.dma("sync", VBs[1024 + t * 128:1024 + (t + 1) * 128, :], vbst[s_].rearrange("p h d -> p (h d)"),
              r=["vbst%d" % s_])
        pA = PS[:, 5, 0:384].bitcast(BF16).rearrange("p (c t) -> p c t", c=6)
        qkr2 = qkr.rearrange("p h d -> p (h d)")
        for c in range(6):
            k.tr(pA[:, c, :], qkr2[:, c * 128:(c + 1) * 128], identb[:], r=["qkr", "identb"], w=["ps5"])
        k.copy("vector", QAT[:, :, tok], pA[:, 0:4, :], r=["ps5"], w=["QAT"])
        k.copy("vector", KAT[:, :, tok], pA[:, 4:6, :], r=["ps5"], w=["KAT"])
        pB = PS[:, 6, :].bitcast(BF16).rearrange("p (c t) -> p c t", c=8)
        qkbr2 = qkbr.rearrange("p h d -> p (h d)")
        for c in range(8):
            k.tr(pB[:, c, :], qkbr2[:, c * 128:(c + 1) * 128], identb[:], r=["qkbr", "identb"], w=["ps6"])
        k.copy("scalar", stT[s_], pB, r=["ps6"], w=["stT"])
        k.dma("sync", QBs[:, tok].rearrange("(c p) t -> p c t", p=128), stT[s_][:, 0:4, :], r=["stT"])
        k.dma("sync", KBs[:, tok].rearrange("(c p) t -> p c t", p=128), stT[s_][:, 4:8, :], r=["stT"])
    k.barrier()

    def epilogue(src65, srckey, osb, rden, obf, slot, row0, col0, ncol):
        k.copy("vector", osb[0:65, 0:ncol], src65, r=[srckey], w=["osb%d" % slot])
        k.op("vector", lambda e: e.reciprocal(out=rden[64:65, 0:ncol], in_=osb[64:65, 0:ncol]), r=["osb%d" % slot], w=["rden%d" % slot])
        k.mm(PS[0:64, 7, 0:ncol], onesf[64:65, 0:64], rden[64:65, 0:ncol], True, True, r=["rden%d" % slot, "onesf"], w=["ps7"])
        k.tt("vector", obf[0:64, 0:ncol], osb[0:64, 0:ncol], PS[0:64, 7, 0:ncol], ALU.mult, r=["osb%d" % slot, "ps7"], w=["obf%d" % slot])
        k.dma("sync", OT[row0:row0 + 64, col0:col0 + ncol], obf[0:64, 0:ncol], r=["obf%d" % slot])

    ar.reset(base12)
    Pb = [ar.get([128, 512], BF16) for _ in range(3)]
    osb = [ar.get([128, 512], F32) for _ in range(2)]
    rden = [ar.get([128, 512], F32) for _ in range(2)]
    obf = [ar.get([128, 512], BF16) for _ in range(2)]
    VA4 = VA.rearrange("p n (k d) -> p n k d", k=2)
    Pb4 = [[Pb[0], Pb[1]], [Pb[2], ar.get([128, 512], BF16)]]
    for pair in range(4 if phases >= 2 else 0):
        kv = pair // 2
        for qb in range(16):
            qs = slice(qb * 512, (qb + 1) * 512)

            def qk_mm(kc):
                for hh in range(2):
                    b0 = 64 * hh
                    sb = 2 * hh + kc % 2
                    k.mm(PS[:, sb, :], KAT[b0:b0 + 64, kv, kc * 128:(kc + 1) * 128], QAT[b0:b0 + 64, pair, qs], True, True,
                         r=["QAT", "KAT"], w=["psS%d" % sb])
            qk_mm(0)
            for kc in range(NT):
                if kc + 1 < NT:
                    qk_mm(kc + 1)
                for hh in range(2):
                    sb = 2 * hh + kc % 2
                    k.act(Pb4[hh][kc % 2], PS[:, sb, :], AF.Exp, r=["psS%d" % sb], w=["Pb%d" % sb])
                for hh in range(2):
                    sb = 2 * hh + kc % 2
                    k.mm(PS[0:65, 4 + hh, :], VA4[:, kc, kv, :], Pb4[hh][kc % 2], kc == 0, kc == NT - 1, r=["VA", "Pb%d" % sb], w=["psO%d" % hh])
            for hh in range(2):
                epilogue(PS[0:65, 4 + hh, :], "psO%d" % hh, osb[hh], rden[hh], obf[hh], hh, (2 * pair + hh) * 64, qb * 512, 512)
    k.barrier()

    ar.reset(0)
    KBT = ar.get([128, S + 2048], BF16)
    QBT = ar.get([128, S], BF16)
    acc = ar.get([128, S], F32)
    vt = [ar.get([128, 65, 130], BF16) for _ in range(2)]
    Pe = [ar.get([128, 512], BF16) for _ in range(3)]
    Pm = [ar.get([128, 512], BF16) for _ in range(3)]
    osb3 = ar.get([128, 512], F32); rden3 = ar.get([128, 512], F32); obf3 = ar.get([128, 512], BF16)
    if phases >= 3:
        k.memset("gpsimd", KBT[:, 0:1024], 0.0, w=["KBT"])
        k.memset("gpsimd", KBT[:, S + 1024:S + 2048], 0.0, w=["KBT"])
    vi = 0
    gi = 0
    for pair in range(4 if phases >= 3 else 0):
        k.dma("sync", KBT[:, 1024:1024 + S], KBs[pair * 128:(pair + 1) * 128, :], w=["KBT"])
        k.dma("sync", QBT[:, :], QBs[pair * 128:(pair + 1) * 128, :], w=["QBT"])
        for hh in range(2):
            h = pair * 2 + hh
            b0 = 64 * hh
            k.memset("gpsimd", acc[0:65, :], 0.0, w=["acc"])
            for Dl in (1, 4, 16):
                L = S // Dl
                nq = L // 128
                for o in range(Dl):
                    vs = vi % 2
                    vi += 1
                    start = 1024 + o - 64 * Dl
                    nch = nq + 1
                    vsrc = VBs[start:start + Dl * (128 * nch - 1) + 1:Dl, pair * 130:(pair + 1) * 130].rearrange("(n p) c -> p n c", p=128)
                    k.dma("sync", vt[vs][:, 0:nch, :], vsrc, r=["VBs"], w=["vt%d" % vs])
                    for c0 in range(0, nq, 2):
                        g = gi % 3
                        gi += 1
                        psS = PS[:, g, :]
                        psO = PS[0:65, 3 + g, 0:256]
                        for cc in range(2):
                            c = c0 + cc
                            q0 = o + Dl * 128 * c
                            qsl = QBT[b0:b0 + 64, q0:q0 + Dl * 127 + 1:Dl]
                            for ty in range(2):
                                n = c + ty
                                k0 = 1024 + o + Dl * (128 * n - 64)
                                ksl = KBT[b0:b0 + 64, k0:k0 + Dl * 127 + 1:Dl]
                                j = cc * 2 + ty
                                k.mm(psS[:, j * 128:(j + 1) * 128], ksl, qsl, True, True, r=["KBT", "QBT"], w=["psS%d" % g])
                        k.act(Pe[g], psS, AF.Exp, r=["psS%d" % g], w=["Pe%d" % g])
                        k.tt("vector", Pm[g], Pe[g], maskAB[:], ALU.mult, r=["Pe%d" % g, "maskAB"], w=["Pm%d" % g])
                        for cc in range(2):
                            c = c0 + cc
                            for ty in range(2):
                                n = c + ty
                                j = cc * 2 + ty
                                k.mm(psO[:, cc * 128:(cc + 1) * 128], vt[vs][:, n, hh * 65:(hh + 1) * 65], Pm[g][:, j * 128:(j + 1) * 128],
                                     ty == 0, ty == 1, r=["vt%d" % vs, "Pm%d" % g], w=["psO%d" % g])
                        q0 = o + Dl * 128 * c0
                        av = acc[0:65, q0:q0 + Dl * 255 + 1:Dl]
                        k.tt("vector", av, psO, av, ALU.add, r=["psO%d" % g, "acc"], w=["acc"])
            for qb in range(16):
                epilogue(acc[0:65, qb * 512:(qb + 1) * 512], "acc", osb3, rden3, obf3, 0, 512 + h * 64, qb * 512, 512)
    k.barrier()

    ar.reset(0)
    wout = ar.get([128, 8, DM], BF16)
    logits = ar.get([128, NT, 36], F32)
    wth = ar.get([128, NT, NE], F32)
    mselp = ar.get([128, NT, NE], BF16)
    base4 = ar.off
    wst2 = ar.get([128, DM], F32)
    for jc in range(8 if phases >= 4 else 0):
        k.dma("sync", wst2, w_out_d[jc * 128:(jc + 1) * 128, :], w=["wst2"])
        k.ts("vector", wout[:, jc, :], wst2, gabc[:, jc:jc + 1], None, ALU.mult, r=["wst2", "gabc"], w=["wout"])
    ot = [ar.get([128, 8, 512], BF16) for _ in range(2)]
    osq = ar.get([128, 8, 128], F32)
    xt4 = ar.get([128, DM], F32)
    hbuf = [ar.get([128, DM], F32) for _ in range(2)]
    xn = ar.get([128, DM], F32)
    xnb = [ar.get([128, DM], BF16) for _ in range(2)]
    xnT = ar.get([128, 8, 128], F32)
    junk4 = ar.get([128, DM], BF16)
    st4 = ar.get([128, 16], F32)
    for t in range(NT if phases >= 4 else 0):
        s_ = t % 2
        tok = slice(t * 128, (t + 1) * 128)
        blk, sub = t // 4, t % 4
        bs = blk % 2
        if sub == 0:
            k.dma("sync", ot[bs], OT[:, blk * 512:(blk + 1) * 512].rearrange("(c p) t -> p c t", p=128), r=["OT"], w=["ot%d" % bs])
        k.dma("sync", xt4, x_d[tok, :], w=["xt4"])
        otl = ot[bs][:, :, sub * 128:(sub + 1) * 128]
        k.tt("gpsimd", osq, otl, otl, ALU.mult, r=["ot%d" % bs], w=["osq"])
        for half in range(2):
            for jc in range(4):
                k.mm(PS[:, 6, half:half + 1], osq[:, half * 4 + jc, :], onesf[:, 0:1], jc == 0, jc == 3, r=["osq", "onesf"], w=["ps6"])
        rstd(st4[:, 2:4], PS[:, 6, 0:2], 512, ["ps6"], ["rab"])
        for half in range(2):
            for part in range(2):
                for jc in range(4):
                    k.mm(PS[:, half * 2 + part, :], otl[:, part * 4 + jc, :], wout[:, part * 4 + jc, half * 512:(half + 1) * 512],
                         jc == 0, jc == 3, r=["ot%d" % bs, "wout"], w=["ps%d" % (half * 2 + part)])
        hb = hbuf[s_]
        for half in range(2):
            hs = slice(half * 512, (half + 1) * 512)
            k.stt("vector", hb[:, hs], PS[:, half * 2, :], st4[:, 2:3], xt4[:, hs], ALU.mult, ALU.add,
                  r=["ps%d" % (half * 2), "rab", "xt4"], w=["hb%d" % s_])
            k.stt("vector", hb[:, hs], PS[:, half * 2 + 1, :], st4[:, 3:4], hb[:, hs], ALU.mult, ALU.add,
                  r=["ps%d" % (half * 2 + 1), "rab", "hb%d" % s_], w=["hb%d" % s_])
        k.dma("sync", Hs[tok, :], hb, r=["hb%d" % s_])
        k.act(junk4, hb, AF.Square, r=["hb%d" % s_], w=["junk4", "ss2"], accum_out=st4[:, 4:5])
        rstd(st4[:, 5:6], st4[:, 4:5], DM, ["ss2"], ["r2"])
        k.stt("vector", xn, hb, st4[:, 5:6], g2bc[:], ALU.mult, ALU.mult, r=["hb%d" % s_, "r2", "g2bc"], w=["xn"])
        pT = PS[:, 4:6, :].rearrange("p a (c t) -> p (a c) t", c=4)
        for kc in range(8):
            k.tr(pT[:, kc, :], xn[:, kc * 128:(kc + 1) * 128], identf[:], r=["xn", "identf"], w=["ps45"])
        k.copy("scalar", xnT, pT, r=["ps45"], w=["xnT"])
        k.copy("gpsimd", xnb[s_], xn, r=["xn"], w=["xnb%d" % s_])
        k.dma("sync", XN[tok, :], xnb[s_], r=["xnb%d" % s_])
        for kc in range(8):
            k.mm(PS[:, 7, 0:36], xnT[:, kc, :], rwsb[:, kc, :], kc == 0, kc == 7, r=["xnT", "rwsb"], w=["ps7"])
        k.tt("vector", logits[:, t, :], PS[:, 7, 0:36], rbbc[:], ALU.add, r=["ps7", "rbbc"], w=["logits"])
    if debug and phases >= 4:
        k.dma("sync", LG[:, :], logits.rearrange("p t n -> p (t n)"), r=["logits"])
    k.barrier()

    ar.reset(base4)
    BIG = 1.0e30
    if phases >= 5:
        V = "vector"
        gl = logits[:, :, 0:4]
        el = logits[:, :, 4:36]
        gm = ar.get([128, NT], F32); ge = ar.get([128, NT, 4], F32); gs = ar.get([128, NT], F32)
        ohg = ar.get([128, NT, 4], F32); pen = ar.get([128, NT, 4], F32)
        em = ar.get([128, NT, 32], F32); em2 = ar.get([128, NT, 32], F32)
        m1 = ar.get([128, NT], F32); m2 = ar.get([128, NT], F32)
        oh1 = ar.get([128, NT, 32], F32); oh2 = ar.get([128, NT, 32], F32)
        p1 = ar.get([128, NT], F32); ga = ar.get([128, NT], F32); gb = ar.get([128, NT], F32)

        def b4(a):
            return a.unsqueeze(2).broadcast_to([128, NT, 4])

        def b32(a):
            return a.unsqueeze(2).broadcast_to([128, NT, 32])
        k.red(V, gm, gl, ALU.max, r=["logits"], w=["gm"])
        k.tt(V, ge, gl, b4(gm), ALU.subtract, r=["logits", "gm"], w=["ge"])
        k.tt(V, ohg, gl, b4(gm), ALU.is_equal, r=["logits", "gm"], w=["ohg"])
        k.act(ge, ge, AF.Exp, r=["ge"], w=["ge"])
        k.red(V, gs, ge, ALU.add, r=["ge"], w=["gs"])
        k.op(V, lambda e: e.reciprocal(out=gs, in_=gs), r=["gs"], w=["gs"])
        k.ts(V, pen, ohg, BIG, -BIG, ALU.mult, ALU.add, r=["ohg"], w=["pen"])
        k.tt(V, em.rearrange("p t (g e) -> p t g e", g=4), el.rearrange("p t (g e) -> p t g e", g=4),
             pen.unsqueeze(3).broadcast_to([128, NT, 4, 8]), ALU.add, r=["logits", "pen"], w=["em"])
        k.red(V, m1, em, ALU.max, r=["em"], w=["m1"])
        k.tt(V, oh1, em, b32(m1), ALU.is_equal, r=["em", "m1"], w=["oh1"])
        k.stt(V, em2, oh1, -BIG, em, ALU.mult, ALU.add, r=["oh1", "em"], w=["em2"])
        k.red(V, m2, em2, ALU.max, r=["em2"], w=["m2"])
        k.tt(V, oh2, em2, b32(m2), ALU.is_equal, r=["em2", "m2"], w=["oh2"])
        k.tt(V, p1, m2, m1, ALU.subtract, r=["m1", "m2"], w=["p1"])
        k.act(p1, p1, AF.Exp, r=["p1"], w=["p1"])
        k.ts(V, p1, p1, 1.0, None, ALU.add, r=["p1"], w=["p1"])
        k.op(V, lambda e: e.reciprocal(out=p1, in_=p1), r=["p1"], w=["p1"])
        k.tt(V, mselp, oh1, oh2, ALU.add, r=["oh1", "oh2"], w=["mselp"])
        msel2 = mselp.rearrange("p t e -> p (t e)")
        wth2 = wth.rearrange("p t e -> p (t e)")
        for q in range(4):
            k.mm(PS[:, q, :], triU[:], msel2[:, q * 512:(q + 1) * 512], True, True, r=["mselp", "triU"], w=["ps%d" % q])
            k.copy("scalar", wth2[:, q * 512:(q + 1) * 512], PS[:, q, :], r=["ps%d" % q], w=["wth"])
        k.tt(V, ga, gs, p1, ALU.mult, r=["gs", "p1"], w=["ga"])
        k.tt(V, gb, gs, ga, ALU.subtract, r=["gs", "ga"], w=["gb"])
        k.tt(V, oh1, oh1, b32(ga), ALU.mult, r=["oh1", "ga"], w=["oh1"])
        k.tt(V, oh2, oh2, b32(gb), ALU.mult, r=["oh2", "gb"], w=["oh2"])
        k.tt(V, Gfull, oh1, oh2, ALU.add, r=["oh1", "oh2"], w=["Gfull"])
        if debug:
            k.dma("sync", DST[:, :], Gfull.rearrange("p t e -> p (t e)"), r=["Gfull"])
    k.barrier()

    EG = [(g * 4, 4) for g in range(8)]
    V = "vector"

    def build_sel(c, selc, gsc=None, pre=""):
        w3 = wth[:, c, :].unsqueeze(2).broadcast_to([128, NE, CT])
        io = iota24[:, :].unsqueeze(1).broadcast_to([128, NE, CT])
        k.tt("vector", selc, w3, io, ALU.is_equal, r=["wth", "iota24"], w=[pre + "selc"])
        k.tt("gpsimd", selc, selc, mselp[:, c, :].unsqueeze(2).broadcast_to([128, NE, CT]), ALU.mult, r=[pre + "selc", "mselp"], w=[pre + "selc"])
        if gsc is not None:
            k.tt("gpsimd", gsc, selc, Gfull[:, c, :].unsqueeze(2).broadcast_to([128, NE, CT]), ALU.mult, r=[pre + "selc", "Gfull"], w=[pre + "gsc"])

    if phases >= 5:
        ar.reset(base4)
        xg = [ar.get([128, DM], BF16) for _ in range(2)]
        selc_ = [ar.get([128, NE, CT], BF16) for _ in range(2)]
        xrow = [ar.get([128, DM], BF16) for _ in range(4)]
        xi = 0
        k.dma("sync", xg[0], XN[0:128, :], r=["XN"], w=["xg0"])
        for c in range(NT):
            s_ = c % 2
            if c + 1 < NT:
                k.dma("sync", xg[1 - s_], XN[(c + 1) * 128:(c + 2) * 128, :], r=["XN"], w=["xg%d" % (1 - s_)])
            build_sel(c, selc_[s_], None, "d%d" % s_)
            sel2 = selc_[s_].rearrange("p e j -> p (e j)")
            for (e0, ne) in EG:
                M = ne * CT
                xr = xi % 4
                xi += 1
                for half in range(2):
                    bk = (xr % 2) * 2 + half
                    k.mm(PS[0:M, bk, :], sel2[:, e0 * CT:e0 * CT + M], xg[s_][:, half * 512:(half + 1) * 512], True, True,
                         r=["d%dselc" % s_, "xg%d" % s_], w=["psd%d" % bk])
                    k.copy("scalar" if half == 0 else "vector", xrow[xr][0:M, half * 512:(half + 1) * 512], PS[0:M, bk, :],
                           r=["psd%d" % bk], w=["xrow%d" % xr])
                for el in range(ne):
                    k.dma("sync", XS[e0 + el, c * CT:(c + 1) * CT, :], xrow[xr][el * CT:(el + 1) * CT, :], r=["xrow%d" % xr])
        k.barrier()

        ar.reset(base4)
        wg = [ar.get([128, 8, 512], BF16) for _ in range(2)]
        wu = [ar.get([128, 8, 512], BF16) for _ in range(2)]
        wd = [ar.get([128, 4, DM], BF16) for _ in range(2)]
        xs = [ar.get([128, 4, DM], BF16) for _ in range(2)]
        xsT = ar.get([128, 8, 512], BF16)
        sg = [ar.get([128, 512], F32) for _ in range(2)]
        hT = ar.get([128, 4, 512], BF16)
        ysb = [ar.get([128, DM], BF16) for _ in range(2)]
        yi = 0
        NBLK = RE // 512
        blocks = [(e_, b_) for e_ in range(NE) for b_ in range(NBLK)]

        def load_w(e_):
            ws = e_ % 2
            k.dma("gpsimd", wg[ws], wg_d[e_].rearrange("(k p) f -> p k f", p=128), w=["wg%d" % ws])
            k.dma("gpsimd", wu[ws], wu_d[e_].rearrange("(k p) f -> p k f", p=128), w=["wu%d" % ws])
            k.dma("gpsimd", wd[ws], wd_d[e_].rearrange("(k p) f -> p k f", p=128), w=["wd%d" % ws])

        def load_x(i):
            e_, b_ = blocks[i]
            k.dma("sync", xs[i % 2], XS[e_, b_ * 512:(b_ + 1) * 512, :].rearrange("(a p) d -> p a d", p=128), r=["XS"], w=["xs%d" % (i % 2)])
        load_w(0)
        load_x(0)
        for i, (e_, blk) in enumerate(blocks):
            ws = e_ % 2
            xb = i % 2
            r0 = blk * 512
            if blk == 0 and e_ + 1 < NE:
                load_w(e_ + 1)
            if i + 1 < len(blocks):
                load_x(i + 1)
            for kc in range(8):
                tb = kc % 2
                pT = PS[:, tb, 0:256].bitcast(BF16).rearrange("p (a t) -> p a t", a=4)
                for a in range(4):
                    k.tr(pT[:, a, :], xs[xb][:, a, kc * 128:(kc + 1) * 128], identb[:], r=["xs%d" % xb, "identb"], w=["pst%d" % tb])
                k.copy("vector" if kc % 2 == 0 else "scalar", xsT[:, kc, :], PS[:, tb, 0:256].bitcast(BF16), r=["pst%d" % tb], w=["xsT"])
            for fc in range(4):
                pb = fc % 2
                for kc in range(8):
                    k.mm(PS[:, 2 + pb, :], wg[ws][:, kc, fc * 128:(fc + 1) * 128], xsT[:, kc, :], kc == 0, kc == 7,
                         r=["wg%d" % ws, "xsT"], w=["psg%d" % pb])
                for kc in range(8):
                    k.mm(PS[:, 4 + pb, :], wu[ws][:, kc, fc * 128:(fc + 1) * 128], xsT[:, kc, :], kc == 0, kc == 7,
                         r=["wu%d" % ws, "xsT"], w=["psu%d" % pb])
                k.act(sg[pb], PS[:, 2 + pb, :], AF.Silu, r=["psg%d" % pb], w=["sg%d" % pb])
                k.tt("vector", hT[:, fc, :], sg[pb], PS[:, 4 + pb, :], ALU.mult, r=["sg%d" % pb, "psu%d" % pb], w=["hT"])
            for rt in range(4):
                ys = yi % 2
                yi += 1
                for dh in range(2):
                    for fc in range(4):
                        k.mm(PS[:, 6 + dh, :], hT[:, fc, rt * 128:(rt + 1) * 128], wd[ws][:, fc, dh * 512:(dh + 1) * 512], fc == 0, fc == 3,
                             r=["hT", "wd%d" % ws], w=["psy%d" % dh])
                k.copy("scalar", ysb[ys][:, 0:512], PS[:, 6, :], r=["psy0"], w=["ysb%d" % ys])
                k.copy("vector", ysb[ys][:, 512:1024], PS[:, 7, :], r=["psy1"], w=["ysb%d" % ys])
                k.dma("sync", Ys[e_, r0 + rt * 128:r0 + (rt + 1) * 128, :], ysb[ys], r=["ysb%d" % ys])
        k.barrier()

        ar.reset(base4)
        selc6 = [ar.get([128, NE, CT], BF16) for _ in range(2)]
        gsc6 = [ar.get([128, NE, CT], F32) for _ in range(2)]
        gt = [ar.get([128, len(EG), 128], F32) for _ in range(2)]
        yr = [ar.get([128, len(EG), DM], F32) for _ in range(2)]
        yrb = [ar.get([128, len(EG), DM], BF16) for _ in range(2)]
        hh_ = [ar.get([128, DM], F32) for _ in range(2)]
        ob = [ar.get([128, DM], F32) for _ in range(2)]
        junk6 = ar.get([128, DM], BF16)
        st6 = ar.get([128, 2 * NT], F32)
        def load6(c):
            s_ = c % 2
            k.dma("sync", hh_[s_], Hs[c * 128:(c + 1) * 128, :], r=["Hs"], w=["hh%d" % s_])
            for gi_, (e0, ne) in enumerate(EG):
                for el in range(ne):
                    k.dma("sync", yrb[s_][el * CT:(el + 1) * CT, gi_, :], Ys[e0 + el, c * CT:(c + 1) * CT, :], r=["Ys"], w=["yrb%d" % s_])
        def prep6(c):
            s_ = c % 2
            build_sel(c, selc6[s_], gsc6[s_], "c%d" % s_)
            gs2 = gsc6[s_].rearrange("p e j -> p (e j)")
            for gi_, (e0, ne) in enumerate(EG):
                M = ne * CT
                bk = gi_ % 2
                k.tr(PS[0:M, bk, 0:128], gs2[:, e0 * CT:e0 * CT + M], identf[:], r=["c%dgsc" % s_, "identf"], w=["pst%d" % bk])
                k.copy("scalar", gt[s_][0:M, gi_, :], PS[0:M, bk, 0:128], r=["pst%d" % bk], w=["gt%d" % s_])
        load6(0)
        prep6(0)
        for c in range(NT):
            s_ = c % 2
            tok = slice(c * 128, (c + 1) * 128)
            if c + 1 < NT:
                load6(c + 1)
            k.copy("scalar", yr[s_][:, 0:4, :], yrb[s_][:, 0:4, :], r=["yrb%d" % s_], w=["yr%d" % s_])
            k.copy("gpsimd", yr[s_][:, 4:8, :], yrb[s_][:, 4:8, :], r=["yrb%d" % s_], w=["yr%d" % s_])
            for half in range(2):
                for gi_, (e0, ne) in enumerate(EG):
                    M = ne * CT
                    k.mm(PS[:, 2 + half, :], gt[s_][0:M, gi_, :], yr[s_][0:M, gi_, half * 512:(half + 1) * 512], gi_ == 0, gi_ == len(EG) - 1,
                         r=["gt%d" % s_, "yr%d" % s_], w=["psc%d" % half])
            if c + 1 < NT:
                prep6(c + 1)
            for half in range(2):
                hs = slice(half * 512, (half + 1) * 512)
                k.tt("vector", hh_[s_][:, hs], hh_[s_][:, hs], PS[:, 2 + half, :], ALU.add, r=["psc%d" % half, "hh%d" % s_], w=["hh%d" % s_])
            k.act(junk6, hh_[s_], AF.Square, r=["hh%d" % s_], w=["junk6", "ss6_%d" % s_], accum_out=st6[:, 2 * c:2 * c + 1])
            rstd(st6[:, 2 * c + 1:2 * c + 2], st6[:, 2 * c:2 * c + 1], DM, ["ss6_%d" % s_], ["r6_%d" % s_])
            k.stt("vector", ob[s_], hh_[s_], st6[:, 2 * c + 1:2 * c + 2], gfbc[:], ALU.mult, ALU.mult,
                  r=["hh%d" % s_, "r6_%d" % s_, "gfbc"], w=["ob%d" % s_])
            k.dma("sync", out_d[tok, :], ob[s_], r=["ob%d" % s_])
    k.barrier()

    sems = {}
    pools = {}
    import contextlib
    with contextlib.ExitStack() as es:
        for e in ENGS:
            sems[e] = es.enter_context(nc.semaphore("s_" + e))
            pools[e] = [es.enter_context(nc.semaphore("d_%s%d" % (e, i))) for i in range(NPOOL)]
        k.emit(sems, pools)
    return nc


def _consts():
    ident = np.eye(128, dtype=np.float32)
    i = np.arange(128)[:, None]; j = np.arange(128)[None, :]
    triu = (i < j).astype(np.float32)
    mA = (j <= i).astype(np.float32)
    mB = (j >= i).astype(np.float32)
    mask = np.concatenate([mA, mB, mA, mB], axis=1)
    return ident, triu, mask


def _tables():
    def inv_freq(dim):
        return (1.0 / (10000.0 ** (np.arange(0, dim, 2, dtype=np.float32) / np.float32(dim)))).astype(np.float32)
    t = np.arange(S, dtype=np.float32)
    row = np.floor(t / 64).astype(np.float32); col = (t % 64).astype(np.float32)
    f = inv_freq(32)
    angA = np.concatenate([row[:, None] * f, col[:, None] * f], axis=-1).astype(np.float32)
    angB = (t[:, None] * inv_freq(64)).astype(np.float32)
    return np.concatenate([np.cos(angA), np.sin(angA), np.cos(angB), np.sin(angB)], axis=-1).astype(np.float32)


def make_in_maps(inp, cores):
    ident, triu, mask = _consts()
    tabs = _tables()
    f = lambda a: np.ascontiguousarray(a, dtype=np.float32)
    shared = {
        "w_in": f(inp["w_in"][0]), "w_out": f(inp["w_out"][0]),
        "g1": f(inp["norm1_g"][0].reshape(8, 128).T),
        "gab": f(np.concatenate([inp["out_norm_a_g"][0], inp["out_norm_b_g"][0]]).reshape(8, 128).T),
        "g2": f(inp["norm2_g"][0]), "gf": f(inp["final_norm_g"]),
        "gq": f(inp["q_norm_g"][0]), "gk": f(inp["k_norm_g"][0]),
        "rw": f(np.concatenate([inp["router_group_w"][0], inp["router_expert_w"][0]], axis=1)),
        "rb": f(np.concatenate([inp["router_group_b"][0], inp["router_expert_b"][0]])),
        "w_gate": f(inp["w_gate"][0]), "w_up": f(inp["w_up"][0]), "w_down": f(inp["w_down"][0]),
        "tabs": tabs, "c_ident": ident, "c_triu": triu, "c_mask": mask,
        "c_e1024": (np.arange(NE) * CAP).astype(np.float32),
        "c_iota": np.arange(CT).astype(np.float32),
    }
    maps = []
    for c in cores:
        m = dict(shared)
        xb = f(inp["x"][c])
        m["x"] = xb
        maps.append(m)
    return maps


def kernel(**inputs):
    inp = {k_: np.asarray(v) for k_, v in inputs.items()}
    nc = build_nc(debug=False)
    maps = make_in_maps(inp, list(range(8)))
    res = run_bass_kernel_spmd(nc, maps, core_ids=list(range(8)))
    out = np.stack([np.asarray(r["out"]) for r in res.results], axis=0)
    return out.astype(np.float32)
```

```python
import numpy as np
import concourse.bass as bass
import concourse.mybir as mybir
from concourse.bass_utils import run_bass_kernel_spmd

F32 = mybir.dt.float32
BF16 = mybir.dt.bfloat16
I32 = mybir.dt.int32
ALU = mybir.AluOpType
AF = mybir.ActivationFunctionType
AX = mybir.AxisListType

S = 8192
NT = 64
DM = 1024
CAP = 1024
CT = 32
RE = NT * CT
NE = 32
EPS = 1e-6
ENGS = ["tensor", "vector", "scalar", "gpsimd", "sync"]
NPOOL = 16
DEBUG = False


class KB:
    def __init__(self, nc):
        self.nc = nc
        self.ops = {e: [] for e in ENGS}
        self.last_w = {}
        self.readers = {}
        self.ndma = {e: 0 for e in ENGS}
        self.dmas = {e: [] for e in ENGS}

    def op(self, eng, fn, r=(), w=(), dma=False):
        rec = dict(eng=eng, fn=fn, deps=[], sig=False, dma=dma, idx=len(self.ops[eng]))
        deps = []
        for k in r:
            if k in self.last_w:
                deps.append(self.last_w[k])
        for k in w:
            if k in self.last_w:
                deps.append(self.last_w[k])
            deps.extend(self.readers.get(k, ()))
        for k in r:
            self.readers.setdefault(k, []).append(rec)
        for k in w:
            self.last_w[k] = rec
            self.readers[k] = []
        if dma:
            rec["k"] = self.ndma[eng]
            self.ndma[eng] += 1
            self.dmas[eng].append(rec)
        seen = set()
        for d in deps:
            if id(d) in seen or d is rec:
                continue
            seen.add(id(d))
            if d["eng"] == "tensor" and eng == "tensor" and not d["dma"]:
                continue
            d["sig"] = True
            rec["deps"].append(d)
        self.ops[eng].append(rec)
        return rec

    def barrier(self):
        lasts = []
        for e in ENGS:
            for rec in reversed(self.ops[e]):
                if rec["fn"] is not None and not rec["dma"]:
                    rec["sig"] = True
                    lasts.append(rec)
                    break
            lasts.extend(self.dmas[e][-NPOOL:])
        for e in ENGS:
            rec = dict(eng=e, fn=None, deps=list(lasts), sig=False, dma=False, idx=len(self.ops[e]))
            self.ops[e].append(rec)
        self.last_w = {}
        self.readers = {}

    def emit(self, sems, pools):
        nc = self.nc
        for e in ENGS:
            c = 0
            for rec in self.ops[e]:
                if rec["dma"]:
                    k = rec["k"]
                    rec["sem"] = pools[e][k % NPOOL]
                    rec["val"] = 16 * (k // NPOOL + 1)
                elif rec["sig"]:
                    c += 1
                    rec["sem"] = sems[e]
                    rec["val"] = c
        with nc.Block() as block:
            for e in ENGS:
                def mk(e):
                    def body(eng):
                        seen = {}
                        def wait(sem, val):
                            key = id(sem)
                            if seen.get(key, 0) >= val:
                                return
                            seen[key] = val
                            eng.wait_ge(sem, val)
                        for rec in self.ops[e]:
                            for d in rec["deps"]:
                                wait(d["sem"], d["val"])
                            if rec["fn"] is None:
                                continue
                            if rec["dma"]:
                                k = rec["k"]
                                if k >= NPOOL:
                                    wait(pools[e][k % NPOOL], 16 * (k // NPOOL))
                                rec["fn"](eng).then_inc(rec["sem"], 16)
                            else:
                                ins = rec["fn"](eng)
                                if rec["sig"]:
                                    ins.then_inc(rec["sem"], 1)
                    return body
                getattr(block, e)(mk(e))

    def mm(self, out, lhsT, rhs, start, stop, r=(), w=()):
        return self.op("tensor", lambda e: e.matmul(out, lhsT=lhsT, rhs=rhs, start=start, stop=stop), r, w)

    def tr(self, out, in_, ident, r=(), w=()):
        return self.op("tensor", lambda e: e.transpose(out, in_, ident), r, w)

    def act(self, out, in_, func, r=(), w=(), **kw):
        return self.op("scalar", lambda e: e.activation(out=out, in_=in_, func=func, **kw), r, w)

    def tt(self, eng, out, in0, in1, op, r=(), w=()):
        return self.op(eng, lambda e: e.tensor_tensor(out=out, in0=in0, in1=in1, op=op), r, w)

    def ts(self, eng, out, in0, s1, s2, op0, op1=None, r=(), w=()):
        if op1 is None:
            return self.op(eng, lambda e: e.tensor_scalar(out=out, in0=in0, scalar1=s1, scalar2=None, op0=op0), r, w)
        return self.op(eng, lambda e: e.tensor_scalar(out=out, in0=in0, scalar1=s1, scalar2=s2, op0=op0, op1=op1), r, w)

    def stt(self, eng, out, in0, scalar, in1, op0, op1, r=(), w=()):
        return self.op(eng, lambda e: e.scalar_tensor_tensor(out=out, in0=in0, scalar=scalar, in1=in1, op0=op0, op1=op1), r, w)

    def copy(self, eng, out, in_, r=(), w=()):
        if eng == "scalar":
            return self.op(eng, lambda e: e.activation(out=out, in_=in_, func=AF.Copy), r, w)
        return self.op(eng, lambda e: e.tensor_copy(out=out, in_=in_), r, w)

    def red(self, eng, out, in_, op, r=(), w=()):
        return self.op(eng, lambda e: e.tensor_reduce(out=out, in_=in_, axis=AX.X, op=op), r, w)

    def memset(self, eng, ap, val, r=(), w=()):
        return self.op(eng, lambda e: e.memset(ap, val), r, w)

    def dma(self, q, out, in_, r=(), w=()):
        return self.op(q, lambda e: e.dma_start(out=out, in_=in_), r, w, dma=True)

    def scatter(self, out, off, in_, bound, r=(), w=()):
        return self.op("gpsimd", lambda e: e.indirect_dma_start(
            out=out, out_offset=bass.IndirectOffsetOnAxis(ap=off, axis=0), in_=in_, in_offset=None,
            bounds_check=bound, oob_is_err=False), r, w, dma=True)

    def gather(self, out, in_, off, bound, r=(), w=()):
        return self.op("gpsimd", lambda e: e.indirect_dma_start(
            out=out, out_offset=None, in_=in_, in_offset=bass.IndirectOffsetOnAxis(ap=off, axis=0),
            bounds_check=bound, oob_is_err=False), r, w, dma=True)


def build_nc(debug=False, phases=6):
    nc = bass.Bass("TRN2", target_bir_lowering=False)
    dk = "ExternalOutput" if debug else "Internal"

    def din(name, shape, dt=F32):
        return nc.dram_tensor(name, list(shape), dt, kind="ExternalInput")

    x_d = din("x", [S, DM])
    w_in_d = din("w_in", [DM, 2304]); w_out_d = din("w_out", [DM, DM])
    g1_d = din("g1", [128, 8]); gab_d = din("gab", [128, 8]); g2_d = din("g2", [DM]); gf_d = din("gf", [DM])
    gq_d = din("gq", [64]); gk_d = din("gk", [64])
    rw_d = din("rw", [DM, 36]); rb_d = din("rb", [36])
    wg_d = din("w_gate", [NE, DM, 512]); wu_d = din("w_up", [NE, DM, 512]); wd_d = din("w_down", [NE, 512, DM])
    tabs_d = din("tabs", [S, 128])
    cid_d = din("c_ident", [128, 128]); ctri_d = din("c_triu", [128, 128]); cmask_d = din("c_mask", [128, 512])
    ce_d = din("c_e1024", [NE])
    cio_d = din("c_iota", [CT])
    out_d = nc.dram_tensor("out", [S, DM], F32, kind="ExternalOutput")

    QBs = nc.dram_tensor("QBs", [512, S], BF16, kind="Internal")
    KBs = nc.dram_tensor("KBs", [512, S], BF16, kind="Internal")
    VBs = nc.dram_tensor("VBs", [S + 2048, 520], BF16, kind="Internal")
    OT = nc.dram_tensor("OT", [DM, S], BF16, kind=dk)
    Hs = nc.dram_tensor("Hs", [S, DM], F32, kind=dk)
    XN = nc.dram_tensor("XN", [S, DM], BF16, kind="Internal")
    XS = nc.dram_tensor("XS", [NE, RE, DM], BF16, kind="Internal")
    Ys = nc.dram_tensor("Ys", [NE, RE, DM], BF16, kind="Internal")
    LG = nc.dram_tensor("LG", [128, NT * 36], F32, kind=dk)
    DST = nc.dram_tensor("DST", [128, NT * NE], F32, kind=dk)

    def A(name, shape, dt):
        t_ = nc.alloc_sbuf_tensor(name, shape, dt)
        return t_[tuple(slice(None) for _ in shape)]
    identb = A("identb", [128, 128], BF16); identf = A("identf", [128, 128], F32)
    maskAB = A("maskAB", [128, 512], BF16); triU = A("triU", [128, 128], BF16)
    onesb = A("onesb", [128, 128], BF16); onesf = A("onesf", [128, 64], F32)
    g1c = A("g1c", [128, 8], F32); gabc = A("gabc", [128, 8], F32)
    gqk = A("gqk", [128, 12, 64], F32)
    g2bc = A("g2bc", [128, DM], F32); gfbc = A("gfbc", [128, DM], F32)
    rwsb = A("rwsb", [128, 8, 36], F32); rbbc = A("rbbc", [128, 36], F32)
    e1024 = A("e1024", [128, NE], F32)
    iota24 = A("iota24", [128, CT], F32)
    Gfull = A("Gfull", [128, NT, NE], F32)
    ARN = 94400
    arena = A("arena", [128, ARN], BF16)
    PS = nc.alloc_psum_tensor("PS", [128, 8, 512], F32)

    class Arena:
        def __init__(self):
            self.off = 0

        def reset(self, off=0):
            self.off = off

        def get(self, shape, dt):
            n = int(np.prod(shape[1:]))
            nb = n * (2 if dt == BF16 else 4)
            nb = (nb + 63) // 64 * 64
            a = self.off // 2
            self.off += nb
            assert self.off <= ARN * 2, ("arena overflow", self.off)
            v = arena[:, a:a + (n if dt == BF16 else 2 * n)]
            if dt != BF16:
                v = v.bitcast(dt)
            if len(shape) == 3:
                v = v.rearrange("p (a b) -> p a b", a=shape[1])
            elif len(shape) == 4:
                v = v.rearrange("p (a b c) -> p a b c", a=shape[1], b=shape[2])
            return v

    ar = Arena()
    k = KB(nc)
    epsc = A("epsc", [128, 1], F32)
    k.memset("vector", epsc[:], EPS, w=["epsc"])

    def rstd(out, in_, n, rk, wk):
        k.act(out, in_, AF.Sqrt, r=rk + ["epsc"], w=wk, scale=1.0 / n, bias=epsc[:, 0:1])
        k.op("vector", lambda e: e.reciprocal(out=out, in_=out), r=wk, w=wk)

    k.dma("gpsimd", identb[:], cid_d[:, :], w=["identb"])
    k.dma("sync", identf[:], cid_d[:, :], w=["identf"])
    k.dma("gpsimd", maskAB[:], cmask_d[:, :], w=["maskAB"])
    k.dma("gpsimd", triU[:], ctri_d[:, :], w=["triU"])
    k.memset("vector", onesb[:], 1.0, w=["onesb"])
    k.memset("vector", onesf[:], 1.0, w=["onesf"])
    k.dma("sync", g1c[:], g1_d[:, :], w=["g1c"])
    k.dma("sync", gabc[:], gab_d[:, :], w=["gabc"])
    for h in range(12):
        src = gq_d if h < 8 else gk_d
        k.dma("sync", gqk[:, h, :], src[:].partition_broadcast(128), w=["gqk"])
    k.ts("vector", gqk[:, 0:8, :], gqk[:, 0:8, :], 0.125, None, ALU.mult, r=["gqk"], w=["gqk"])
    k.dma("sync", g2bc[:], g2_d[:].partition_broadcast(128), w=["g2bc"])
    k.dma("sync", gfbc[:], gf_d[:].partition_broadcast(128), w=["gfbc"])
    k.dma("sync", rwsb[:], rw_d[:, :].rearrange("(k p) n -> p k n", p=128), w=["rwsb"])
    k.dma("sync", rbbc[:], rb_d[:].partition_broadcast(128), w=["rbbc"])
    k.dma("sync", e1024[:], ce_d[:].partition_broadcast(128), w=["e1024"])
    k.dma("sync", iota24[:], cio_d[:].partition_broadcast(128), w=["iota24"])

    win = ar.get([128, 8, 2432], BF16)
    QAT = ar.get([128, 4, S], BF16)
    KAT = ar.get([128, 2, S], BF16)
    VA = ar.get([128, NT, 130], BF16)
    base12 = ar.off

    wst = ar.get([128, 2304], F32)
    zt = ar.get([128, 8, 520], BF16)
    for kc in range(8):
        k.dma("sync", wst, w_in_d[kc * 128:(kc + 1) * 128, :], w=["wst"])
        gcol = g1c[:, kc:kc + 1]
        k.ts("vector", win[:, kc, 0:512], wst[:, 0:512], gcol, None, ALU.mult, r=["wst", "g1c"], w=["win"])
        k.ts("vector", win[:, kc, 512:2048], wst[:, 768:2304], gcol, None, ALU.mult, r=["wst", "g1c"], w=["win"])
        k.ts("gpsimd", win[:, kc, 2048:2304].rearrange("p (a b c) -> p a b c", a=2, b=2),
             wst[:, 512:640].rearrange("p (a b c) -> p a b c", a=2, b=1).broadcast_to([128, 2, 2, 64]),
             gcol, None, ALU.mult, r=["wst", "g1c"], w=["win"])
        k.ts("gpsimd", win[:, kc, 2304:2432], wst[:, 640:768], gcol, None, ALU.mult, r=["wst", "g1c"], w=["win"])
    k.memset("gpsimd", zt, 0.0, w=["zt"])
    k.dma("sync", VBs[0:1024, :].rearrange("(n p) c -> p n c", p=128), zt, r=["zt"])
    k.dma("sync", VBs[S + 1024:S + 2048, :].rearrange("(n p) c -> p n c", p=128), zt, r=["zt"])
    k.memset("gpsimd", VA.rearrange("p n (k d) -> p n k d", k=2)[:, :, :, 64:65], 1.0, w=["VA"])
    k.barrier()

    ar.reset(base12)
    xTb_ = ar.get([128, 8, 128], BF16); xTb = [xTb_, xTb_]
    xt = ar.get([128, DM], F32)
    junk = ar.get([128, DM], BF16)
    tab = [ar.get([128, 128], F32) for _ in range(2)]
    qk = ar.get([128, 12, 64], F32)
    sq = ar.get([128, 12, 64], F32)
    qkr = ar.get([128, 12, 64], BF16)
    t1 = ar.get([128, 12, 32], F32); t2 = ar.get([128, 12, 32], F32)
    qkb = ar.get([128, 16, 64], F32)
    qkbr = ar.get([128, 16, 64], BF16)
    u1 = ar.get([128, 16, 32], F32); u2 = ar.get([128, 16, 32], F32)
    vbst = [ar.get([128, 8, 65], BF16) for _ in range(2)]
    stT_ = ar.get([128, 8, 128], BF16); stT = [stT_, stT_]
    st1 = ar.get([128, 32], F32)
    for s_ in range(2):
        k.memset("gpsimd", vbst[s_][:, :, 64:65], 1.0, w=["vbst%d" % s_])

    def rotary(eng, src, dst, ta, tb, cs, sn, nh, kr, kw, pre):
        C = cs.unsqueeze(1).broadcast_to([128, nh, 32]); Sn = sn.unsqueeze(1).broadcast_to([128, nh, 32])
        x1 = src[:, :, 0:32]; x2 = src[:, :, 32:64]
        k.tt(eng, ta, x1, C, ALU.mult, r=kr, w=[pre + "a"])
        k.tt(eng, tb, x2, Sn, ALU.mult, r=kr, w=[pre + "b"])
        k.tt(eng, dst[:, :, 0:32], ta, tb, ALU.subtract, r=[pre + "a", pre + "b"], w=kw)
        k.tt(eng, ta, x2, C, ALU.mult, r=kr + kw, w=[pre + "a"])
        k.tt(eng, tb, x1, Sn, ALU.mult, r=kr + kw, w=[pre + "b"])
        k.tt(eng, dst[:, :, 32:64], ta, tb, ALU.add, r=[pre + "a", pre + "b"], w=kw)

    def stageA(t):
        s_ = t % 2
        tok = slice(t * 128, (t + 1) * 128)
        rs = st1[:, 1:2]
        k.dma("sync", xt, x_d[tok, :], w=["xt"])
        k.dma("sync", tab[s_], tabs_d[tok, :], w=["tab%d" % s_])
        sqj = sq.rearrange("p h d -> p (h d)").bitcast(BF16)[:, 0:DM]
        k.act(sqj, xt, AF.Square, r=["xt"], w=["sq", "ss"], accum_out=st1[:, 0:1])
        k.copy("gpsimd", junk, xt, r=["xt"], w=["junk"])
        pX = PS[:, 7, :].bitcast(BF16).rearrange("p (c t) -> p c t", c=8)
        for kc in range(8):
            k.tr(pX[:, kc, :], junk[:, kc * 128:(kc + 1) * 128], identb[:], r=["junk", "identb"], w=["ps7"])
        k.copy("vector", xTb[s_], pX, r=["ps7"], w=["xTb"])
        rstd(st1[:, 1:2], st1[:, 0:1], DM, ["ss"], ["rstd1"])
        for b in range(5):
            n0, n1 = b * 512, min((b + 1) * 512, 2432)
            for kc in range(8):
                k.mm(PS[:, b, 0:n1 - n0], xTb[s_][:, kc, :], win[:, kc, n0:n1], kc == 0, kc == 7,
                     r=["xTb", "win"], w=["ps%d" % b])

    def stageB1(t):
        s_ = t % 2
        tok = slice(t * 128, (t + 1) * 128)
        rs = st1[:, 1:2]
        k.act(qk[:, 0:8, :], PS[:, 0, :].rearrange("p (h d) -> p h d", h=8), AF.Copy, r=["ps0", "rstd1"], w=["qk"], scale=rs)
        k.act(qk[:, 8:12, :], PS[:, 4, 0:256].rearrange("p (h d) -> p h d", h=4), AF.Copy, r=["ps4", "rstd1"], w=["qk"], scale=rs)
        k.act(VA[:, t, :].rearrange("p (k d) -> p k d", k=2)[:, :, 0:64], PS[:, 4, 256:384].rearrange("p (k d) -> p k d", k=2),
              AF.Copy, r=["ps4", "rstd1"], w=["VA"], scale=rs)
        k.act(qkb[:, 0:8, :], PS[:, 1, :].rearrange("p (h d) -> p h d", h=8), AF.Copy, r=["ps1", "rstd1"], w=["qkb"], scale=rs)
        k.act(qkb[:, 8:16, :], PS[:, 2, :].rearrange("p (h d) -> p h d", h=8), AF.Copy, r=["ps2", "rstd1"], w=["qkb"], scale=rs)
        k.act(vbst[s_][:, :, 0:64], PS[:, 3, :].rearrange("p (h d) -> p h d", h=8), AF.Copy,
              r=["ps3", "rstd1"], w=["vbst%d" % s_], scale=rs)

    def stageB2(t):
        s_ = t % 2
        tok = slice(t * 128, (t + 1) * 128)
        rs = st1[:, 1:2]
        k.tt("vector", sq, qk, qk, ALU.mult, r=["qk"], w=["sq"])
        k.red("vector", st1[:, 2:14], sq, ALU.add, r=["sq"], w=["ssq"])
        rstd(st1[:, 14:26], st1[:, 2:14], 64, ["ssq"], ["rq"])
        k.tt("vector", qk, qk, st1[:, 14:26].unsqueeze(2).broadcast_to([128, 12, 64]), ALU.mult, r=["qk", "rq"], w=["qk"])
        k.tt("vector", qk, qk, gqk, ALU.mult, r=["qk", "gqk"], w=["qk"])
        rotary("vector", qk, qkr, t1, t2, tab[s_][:, 0:32], tab[s_][:, 32:64], 12, ["qk", "tab%d" % s_], ["qkr"], "t")
        k.ts("gpsimd", qkb[:, 0:8, :], qkb[:, 0:8, :], 0.125, None, ALU.mult, r=["qkb"], w=["qkb"])
        rotary("gpsimd", qkb, qkbr, u1, u2, tab[s_][:, 64:96], tab[s_][:, 96:128], 16, ["qkb", "tab%d" % s_], ["qkbr"], "u")
        k.dma("sync", VBs[1024 + t * 128:1024 + (t + 1) * 128, :], vbst[s_].rearrange("p h d -> p (h d)"),
              r=["vbst%d" % s_])
        pA = PS[:, 5, 0:384].bitcast(BF16).rearrange("p (c t) -> p c t", c=6)
        qkr2 = qkr.rearrange("p h d -> p (h d)")
        for c in range(6):
            k.tr(pA[:, c, :], qkr2[:, c * 128:(c + 1) * 128], identb[:], r=["qkr", "identb"], w=["ps5"])
        k.copy("vector", QAT[:, :, tok], pA[:, 0:4, :], r=["ps5"], w=["QAT"])
        k.copy("vector", KAT[:, :, tok], pA[:, 4:6, :], r=["ps5"], w=["KAT"])
        pB = PS[:, 6, :].bitcast(BF16).rearrange("p (c t) -> p c t", c=8)
        qkbr2 = qkbr.rearrange("p h d -> p (h d)")
        for c in range(8):
            k.tr(pB[:, c, :], qkbr2[:, c * 128:(c + 1) * 128], identb[:], r=["qkbr", "identb"], w=["ps6"])
        k.copy("scalar", stT[s_], pB, r=["ps6"], w=["stT"])
        k.dma("sync", QBs[:, tok].rearrange("(c p) t -> p c t", p=128), stT[s_][:, 0:4, :], r=["stT"])
        k.dma("sync", KBs[:, tok].rearrange("(c p) t -> p c t", p=128), stT[s_][:, 4:8, :], r=["stT"])

    if phases >= 1:
        stageA(0)
        for t in range(NT):
            stageB1(t)
            if t + 1 < NT:
                stageA(t + 1)
            stageB2(t)
    k.barrier()

    def epilogue(src65, srckey, osb, rden, obf, slot, row0, col0, ncol):
        k.copy("vector", osb[0:65, 0:ncol], src65, r=[srckey], w=["osb%d" % slot])
        k.op("vector", lambda e: e.reciprocal(out=rden[64:65, 0:ncol], in_=osb[64:65, 0:ncol]), r=["osb%d" % slot], w=["rden%d" % slot])
        k.mm(PS[0:64, 7, 0:ncol], onesf[64:65, 0:64], rden[64:65, 0:ncol], True, True, r=["rden%d" % slot, "onesf"], w=["ps7"])
        k.tt("vector", obf[0:64, 0:ncol], osb[0:64, 0:ncol], PS[0:64, 7, 0:ncol], ALU.mult, r=["osb%d" % slot, "ps7"], w=["obf%d" % slot])
        k.dma("sync", OT[row0:row0 + 64, col0:col0 + ncol], obf[0:64, 0:ncol], r=["obf%d" % slot])

    ar.reset(base12)
    Pb = [ar.get([128, 512], BF16) for _ in range(3)]
    osb = [ar.get([128, 512], F32) for _ in range(2)]
    rden = [ar.get([128, 512], F32) for _ in range(2)]
    obf = [ar.get([128, 512], BF16) for _ in range(2)]
    VA4 = VA.rearrange("p n (k d) -> p n k d", k=2)
    Pb4 = [[Pb[0], Pb[1]], [Pb[2], ar.get([128, 512], BF16)]]
    for pair in range(4 if phases >= 2 else 0):
        kv = pair // 2
        for qb in range(16):
            qs = slice(qb * 512, (qb + 1) * 512)

            def qk_mm(kc):
                for hh in range(2):
                    b0 = 64 * hh
                    sb = 2 * hh + kc % 2
                    k.mm(PS[:, sb, :], KAT[b0:b0 + 64, kv, kc * 128:(kc + 1) * 128], QAT[b0:b0 + 64, pair, qs], True, True,
                         r=["QAT", "KAT"], w=["psS%d" % sb])
            qk_mm(0)
            for kc in range(NT):
                if kc + 1 < NT:
                    qk_mm(kc + 1)
                for hh in range(2):
                    sb = 2 * hh + kc % 2
                    k.act(Pb4[hh][kc % 2], PS[:, sb, :], AF.Exp, r=["psS%d" % sb], w=["Pb%d" % sb])
                for hh in range(2):
                    sb = 2 * hh + kc % 2
                    k.mm(PS[0:65, 4 + hh, :], VA4[:, kc, kv, :], Pb4[hh][kc % 2], kc == 0, kc == NT - 1, r=["VA", "Pb%d" % sb], w=["psO%d" % hh])
            for hh in range(2):
                epilogue(PS[0:65, 4 + hh, :], "psO%d" % hh, osb[hh], rden[hh], obf[hh], hh, (2 * pair + hh) * 64, qb * 512, 512)
    k.barrier()

    ar.reset(0)
    KBT = ar.get([128, S + 2048], BF16)
    QBT = ar.get([128, S], BF16)
    acc = ar.get([128, S], F32)
    vt = [ar.get([128, 65, 130], BF16) for _ in range(2)]
    Pe = [ar.get([128, 512], BF16) for _ in range(3)]
    Pm = [ar.get([128, 512], BF16) for _ in range(3)]
    osb3 = ar.get([128, 512], F32); rden3 = ar.get([128, 512], F32); obf3 = ar.get([128, 512], BF16)
    if phases >= 3:
        k.memset("gpsimd", KBT[:, 0:1024], 0.0, w=["KBT"])
        k.memset("gpsimd", KBT[:, S + 1024:S + 2048], 0.0, w=["KBT"])
    vi = 0
    gi = 0
    for pair in range(4 if phases >= 3 else 0):
        k.dma("sync", KBT[:, 1024:1024 + S], KBs[pair * 128:(pair + 1) * 128, :], w=["KBT"])
        k.dma("sync", QBT[:, :], QBs[pair * 128:(pair + 1) * 128, :], w=["QBT"])
        for hh in range(2):
            h = pair * 2 + hh
            b0 = 64 * hh
            k.memset("gpsimd", acc[0:65, :], 0.0, w=["acc"])
            for Dl in (1, 4, 16):
                L = S // Dl
                nq = L // 128
                for o in range(Dl):
                    vs = vi % 2
                    vi += 1
                    start = 1024 + o - 64 * Dl
                    nch = nq + 1
                    vsrc = VBs[start:start + Dl * (128 * nch - 1) + 1:Dl, pair * 130:(pair + 1) * 130].rearrange("(n p) c -> p n c", p=128)
                    k.dma("sync", vt[vs][:, 0:nch, :], vsrc, r=["VBs"], w=["vt%d" % vs])
                    for c0 in range(0, nq, 2):
                        g = gi % 3
                        gi += 1
                        psS = PS[:, g, :]
                        psO = PS[0:65, 3 + g, 0:256]
                        for cc in range(2):
                            c = c0 + cc
                            q0 = o + Dl * 128 * c
                            qsl = QBT[b0:b0 + 64, q0:q0 + Dl * 127 + 1:Dl]
                            for ty in range(2):
                                n = c + ty
                                k0 = 1024 + o + Dl * (128 * n - 64)
                                ksl = KBT[b0:b0 + 64, k0:k0 + Dl * 127 + 1:Dl]
                                j = cc * 2 + ty
                                k.mm(psS[:, j * 128:(j + 1) * 128], ksl, qsl, True, True, r=["KBT", "QBT"], w=["psS%d" % g])
                        k.act(Pe[g], psS, AF.Exp, r=["psS%d" % g], w=["Pe%d" % g])
                        k.tt("vector", Pm[g], Pe[g], maskAB[:], ALU.mult, r=["Pe%d" % g, "maskAB"], w=["Pm%d" % g])
                        for cc in range(2):
                            c = c0 + cc
                            for ty in range(2):
                                n = c + ty
                                j = cc * 2 + ty
                                k.mm(psO[:, cc * 128:(cc + 1) * 128], vt[vs][:, n, hh * 65:(hh + 1) * 65], Pm[g][:, j * 128:(j + 1) * 128],
                                     ty == 0, ty == 1, r=["vt%d" % vs, "Pm%d" % g], w=["psO%d" % g])
                        q0 = o + Dl * 128 * c0
                        av = acc[0:65, q0:q0 + Dl * 255 + 1:Dl]
                        k.tt("vector", av, psO, av, ALU.add, r=["psO%d" % g, "acc"], w=["acc"])
            for qb in range(16):
                epilogue(acc[0:65, qb * 512:(qb + 1) * 512], "acc", osb3, rden3, obf3, 0, 512 + h * 64, qb * 512, 512)
    k.barrier()

    ar.reset(0)
    wout = ar.get([128, 8, DM], BF16)
    logits = ar.get([128, NT, 36], F32)
    wth = ar.get([128, NT, NE], F32)
    mselp = ar.get([128, NT, NE], BF16)
    base4 = ar.off
    wst2 = ar.get([128, DM], F32)
    for jc in range(8 if phases >= 4 else 0):
        k.dma("sync", wst2, w_out_d[jc * 128:(jc + 1) * 128, :], w=["wst2"])
        k.ts("vector", wout[:, jc, :], wst2, gabc[:, jc:jc + 1], None, ALU.mult, r=["wst2", "gabc"], w=["wout"])
    ot = [ar.get([128, 8, 512], BF16) for _ in range(2)]
    osq = ar.get([128, 8, 128], F32)
    xt4 = ar.get([128, DM], F32)
    hbuf = [ar.get([128, DM], F32) for _ in range(2)]
    xn = ar.get([128, DM], F32)
    xnb = [ar.get([128, DM], BF16) for _ in range(2)]
    xnT = ar.get([128, 8, 128], F32)
    junk4 = ar.get([128, DM], BF16)
    st4 = ar.get([128, 16], F32)
    for t in range(NT if phases >= 4 else 0):
        s_ = t % 2
        tok = slice(t * 128, (t + 1) * 128)
        blk, sub = t // 4, t % 4
        bs = blk % 2
        if sub == 0:
            k.dma("sync", ot[bs], OT[:, blk * 512:(blk + 1) * 512].rearrange("(c p) t -> p c t", p=128), r=["OT"], w=["ot%d" % bs])
        k.dma("sync", xt4, x_d[tok, :], w=["xt4"])
        otl = ot[bs][:, :, sub * 128:(sub + 1) * 128]
        k.tt("gpsimd", osq, otl, otl, ALU.mult, r=["ot%d" % bs], w=["osq"])
        for half in range(2):
            for jc in range(4):
                k.mm(PS[:, 6, half:half + 1], osq[:, half * 4 + jc, :], onesf[:, 0:1], jc == 0, jc == 3, r=["osq", "onesf"], w=["ps6"])
        rstd(st4[:, 2:4], PS[:, 6, 0:2], 512, ["ps6"], ["rab"])
        for half in range(2):
            for part in range(2):
                for jc in range(4):
                    k.mm(PS[:, half * 2 + part, :], otl[:, part * 4 + jc, :], wout[:, part * 4 + jc, half * 512:(half + 1) * 512],
                         jc == 0, jc == 3, r=["ot%d" % bs, "wout"], w=["ps%d" % (half * 2 + part)])
        hb = hbuf[s_]
        for half in range(2):
            hs = slice(half * 512, (half + 1) * 512)
            k.stt("vector", hb[:, hs], PS[:, half * 2, :], st4[:, 2:3], xt4[:, hs], ALU.mult, ALU.add,
                  r=["ps%d" % (half * 2), "rab", "xt4"], w=["hb%d" % s_])
            k.stt("vector", hb[:, hs], PS[:, half * 2 + 1, :], st4[:, 3:4], hb[:, hs], ALU.mult, ALU.add,
                  r=["ps%d" % (half * 2 + 1), "rab", "hb%d" % s_], w=["hb%d" % s_])
        k.dma("sync", Hs[tok, :], hb, r=["hb%d" % s_])
        k.act(junk4, hb, AF.Square, r=["hb%d" % s_], w=["junk4", "ss2"], accum_out=st4[:, 4:5])
        rstd(st4[:, 5:6], st4[:, 4:5], DM, ["ss2"], ["r2"])
        k.stt("vector", xn, hb, st4[:, 5:6], g2bc[:], ALU.mult, ALU.mult, r=["hb%d" % s_, "r2", "g2bc"], w=["xn"])
        pT = PS[:, 4:6, :].rearrange("p a (c t) -> p (a c) t", c=4)
        for kc in range(8):
            k.tr(pT[:, kc, :], xn[:, kc * 128:(kc + 1) * 128], identf[:], r=["xn", "identf"], w=["ps45"])
        k.copy("scalar", xnT, pT, r=["ps45"], w=["xnT"])
        k.copy("gpsimd", xnb[s_], xn, r=["xn"], w=["xnb%d" % s_])
        k.dma("sync", XN[tok, :], xnb[s_], r=["xnb%d" % s_])
        for kc in range(8):
            k.mm(PS[:, 7, 0:36], xnT[:, kc, :], rwsb[:, kc, :], kc == 0, kc == 7, r=["xnT", "rwsb"], w=["ps7"])
        k.tt("vector", logits[:, t, :], PS[:, 7, 0:36], rbbc[:], ALU.add, r=["ps7", "rbbc"], w=["logits"])
    if debug and phases >= 4:
        k.dma("sync", LG[:, :], logits.rearrange("p t n -> p (t n)"), r=["logits"])
    k.barrier()

    ar.reset(base4)
    BIG = 1.0e30
    if phases >= 5:
        V = "vector"
        gl = logits[:, :, 0:4]
        el = logits[:, :, 4:36]
        gm = ar.get([128, NT], F32); ge = ar.get([128, NT, 4], F32); gs = ar.get([128, NT], F32)
        ohg = ar.get([128, NT, 4], F32); pen = ar.get([128, NT, 4], F32)
        em = ar.get([128, NT, 32], F32); em2 = ar.get([128, NT, 32], F32)
        m1 = ar.get([128, NT], F32); m2 = ar.get([128, NT], F32)
        oh1 = ar.get([128, NT, 32], F32); oh2 = ar.get([128, NT, 32], F32)
        p1 = ar.get([128, NT], F32); ga = ar.get([128, NT], F32); gb = ar.get([128, NT], F32)

        def b4(a):
            return a.unsqueeze(2).broadcast_to([128, NT, 4])

        def b32(a):
            return a.unsqueeze(2).broadcast_to([128, NT, 32])
        k.red(V, gm, gl, ALU.max, r=["logits"], w=["gm"])
        k.tt(V, ge, gl, b4(gm), ALU.subtract, r=["logits", "gm"], w=["ge"])
        k.tt(V, ohg, gl, b4(gm), ALU.is_equal, r=["logits", "gm"], w=["ohg"])
        k.act(ge, ge, AF.Exp, r=["ge"], w=["ge"])
        k.red(V, gs, ge, ALU.add, r=["ge"], w=["gs"])
        k.op(V, lambda e: e.reciprocal(out=gs, in_=gs), r=["gs"], w=["gs"])
        k.ts(V, pen, ohg, BIG, -BIG, ALU.mult, ALU.add, r=["ohg"], w=["pen"])
        k.tt(V, em.rearrange("p t (g e) -> p t g e", g=4), el.rearrange("p t (g e) -> p t g e", g=4),
             pen.unsqueeze(3).broadcast_to([128, NT, 4, 8]), ALU.add, r=["logits", "pen"], w=["em"])
        k.red(V, m1, em, ALU.max, r=["em"], w=["m1"])
        k.tt(V, oh1, em, b32(m1), ALU.is_equal, r=["em", "m1"], w=["oh1"])
        k.stt(V, em2, oh1, -BIG, em, ALU.mult, ALU.add, r=["oh1", "em"], w=["em2"])
        k.red(V, m2, em2, ALU.max, r=["em2"], w=["m2"])
        k.tt(V, oh2, em2, b32(m2), ALU.is_equal, r=["em2", "m2"], w=["oh2"])
        k.tt(V, p1, m2, m1, ALU.subtract, r=["m1", "m2"], w=["p1"])
        k.act(p1, p1, AF.Exp, r=["p1"], w=["p1"])
        k.ts(V, p1, p1, 1.0, None, ALU.add, r=["p1"], w=["p1"])
        k.op(V, lambda e: e.reciprocal(out=p1, in_=p1), r=["p1"], w=["p1"])
        k.tt(V, mselp, oh1, oh2, ALU.add, r=["oh1", "oh2"], w=["mselp"])
        msel2 = mselp.rearrange("p t e -> p (t e)")
        wth2 = wth.rearrange("p t e -> p (t e)")
        for q in range(4):
            k.mm(PS[:, q, :], triU[:], msel2[:, q * 512:(q + 1) * 512], True, True, r=["mselp", "triU"], w=["ps%d" % q])
            k.copy("scalar", wth2[:, q * 512:(q + 1) * 512], PS[:, q, :], r=["ps%d" % q], w=["wth"])
        k.tt(V, ga, gs, p1, ALU.mult, r=["gs", "p1"], w=["ga"])
        k.tt(V, gb, gs, ga, ALU.subtract, r=["gs", "ga"], w=["gb"])
        k.tt(V, oh1, oh1, b32(ga), ALU.mult, r=["oh1", "ga"], w=["oh1"])
        k.tt(V, oh2, oh2, b32(gb), ALU.mult, r=["oh2", "gb"], w=["oh2"])
        k.tt(V, Gfull, oh1, oh2, ALU.add, r=["oh1", "oh2"], w=["Gfull"])
        if debug:
            k.dma("sync", DST[:, :], Gfull.rearrange("p t e -> p (t e)"), r=["Gfull"])
    k.barrier()

    EG = [(g * 4, 4) for g in range(8)]
    V = "vector"

    def build_sel(c, selc, gsc=None, pre=""):
        w3 = wth[:, c, :].unsqueeze(2).broadcast_to([128, NE, CT])
        io = iota24[:, :].unsqueeze(1).broadcast_to([128, NE, CT])
        k.tt("vector", selc, w3, io, ALU.is_equal, r=["wth", "iota24"], w=[pre + "selc"])
        k.tt("gpsimd", selc, selc, mselp[:, c, :].unsqueeze(2).broadcast_to([128, NE, CT]), ALU.mult, r=[pre + "selc", "mselp"], w=[pre + "selc"])
        if gsc is not None:
            k.tt("gpsimd", gsc, selc, Gfull[:, c, :].unsqueeze(2).broadcast_to([128, NE, CT]), ALU.mult, r=[pre + "selc", "Gfull"], w=[pre + "gsc"])

    if phases >= 5:
        ar.reset(base4)
        xg = [ar.get([128, DM], BF16) for _ in range(2)]
        selc_ = [ar.get([128, NE, CT], BF16) for _ in range(2)]
        xrow = [ar.get([128, DM], BF16) for _ in range(4)]
        xi = 0
        k.dma("sync", xg[0], XN[0:128, :], r=["XN"], w=["xg0"])
        for c in range(NT):
            s_ = c % 2
            if c + 1 < NT:
                k.dma("sync", xg[1 - s_], XN[(c + 1) * 128:(c + 2) * 128, :], r=["XN"], w=["xg%d" % (1 - s_)])
            build_sel(c, selc_[s_], None, "d%d" % s_)
            sel2 = selc_[s_].rearrange("p e j -> p (e j)")
            for (e0, ne) in EG:
                M = ne * CT
                xr = xi % 4
                xi += 1
                for half in range(2):
                    bk = (xr % 2) * 2 + half
                    k.mm(PS[0:M, bk, :], sel2[:, e0 * CT:e0 * CT + M], xg[s_][:, half * 512:(half + 1) * 512], True, True,
                         r=["d%dselc" % s_, "xg%d" % s_], w=["psd%d" % bk])
                    k.copy("scalar" if half == 0 else "vector", xrow[xr][0:M, half * 512:(half + 1) * 512], PS[0:M, bk, :],
                           r=["psd%d" % bk], w=["xrow%d" % xr])
                for el in range(ne):
                    k.dma("sync", XS[e0 + el, c * CT:(c + 1) * CT, :], xrow[xr][el * CT:(el + 1) * CT, :], r=["xrow%d" % xr])
        k.barrier()

        ar.reset(base4)
        wg = [ar.get([128, 8, 512], BF16) for _ in range(2)]
        wu = [ar.get([128, 8, 512], BF16) for _ in range(2)]
        wd = [ar.get([128, 4, DM], BF16) for _ in range(2)]
        xs = [ar.get([128, 4, DM], BF16) for _ in range(2)]
        xsT = ar.get([128, 8, 512], BF16)
        sg = [ar.get([128, 512], F32) for _ in range(2)]
        hT = ar.get([128, 4, 512], BF16)
        ysb = [ar.get([128, DM], BF16) for _ in range(2)]
        yi = 0
        NBLK = RE // 512
        blocks = [(e_, b_) for e_ in range(NE) for b_ in range(NBLK)]

        def load_w(e_):
            ws = e_ % 2
            k.dma("gpsimd", wg[ws], wg_d[e_].rearrange("(k p) f -> p k f", p=128), w=["wg%d" % ws])
            k.dma("gpsimd", wu[ws], wu_d[e_].rearrange("(k p) f -> p k f", p=128), w=["wu%d" % ws])
            k.dma("gpsimd", wd[ws], wd_d[e_].rearrange("(k p) f -> p k f", p=128), w=["wd%d" % ws])

        def load_x(i):
            e_, b_ = blocks[i]
            k.dma("sync", xs[i % 2], XS[e_, b_ * 512:(b_ + 1) * 512, :].rearrange("(a p) d -> p a d", p=128), r=["XS"], w=["xs%d" % (i % 2)])
        load_w(0)
        load_x(0)
        for i, (e_, blk) in enumerate(blocks):
            ws = e_ % 2
            xb = i % 2
            r0 = blk * 512
            if blk == 0 and e_ + 1 < NE:
                load_w(e_ + 1)
            if i + 1 < len(blocks):
                load_x(i + 1)
            for kc in range(8):
                tb = kc % 2
                pT = PS[:, tb, 0:256].bitcast(BF16).rearrange("p (a t) -> p a t", a=4)
                for a in range(4):
                    k.tr(pT[:, a, :], xs[xb][:, a, kc * 128:(kc + 1) * 128], identb[:], r=["xs%d" % xb, "identb"], w=["pst%d" % tb])
                k.copy("vector" if kc % 2 == 0 else "scalar", xsT[:, kc, :], PS[:, tb, 0:256].bitcast(BF16), r=["pst%d" % tb], w=["xsT"])
            for fc in range(4):
                pb = fc % 2
                for kc in range(8):
                    k.mm(PS[:, 2 + pb, :], wg[ws][:, kc, fc * 128:(fc + 1) * 128], xsT[:, kc, :], kc == 0, kc == 7,
                         r=["wg%d" % ws, "xsT"], w=["psg%d" % pb])
                for kc in range(8):
                    k.mm(PS[:, 4 + pb, :], wu[ws][:, kc, fc * 128:(fc + 1) * 128], xsT[:, kc, :], kc == 0, kc == 7,
                         r=["wu%d" % ws, "xsT"], w=["psu%d" % pb])
                k.act(sg[pb], PS[:, 2 + pb, :], AF.Silu, r=["psg%d" % pb], w=["sg%d" % pb])
                k.tt("vector", hT[:, fc, :], sg[pb], PS[:, 4 + pb, :], ALU.mult, r=["sg%d" % pb, "psu%d" % pb], w=["hT"])
            for rt in range(4):
                ys = yi % 2
                yi += 1
                for dh in range(2):
                    for fc in range(4):
                        k.mm(PS[:, 6 + dh, :], hT[:, fc, rt * 128:(rt + 1) * 128], wd[ws][:, fc, dh * 512:(dh + 1) * 512], fc == 0, fc == 3,
                             r=["hT", "wd%d" % ws], w=["psy%d" % dh])
                k.copy("scalar", ysb[ys][:, 0:512], PS[:, 6, :], r=["psy0"], w=["ysb%d" % ys])
                k.copy("vector", ysb[ys][:, 512:1024], PS[:, 7, :], r=["psy1"], w=["ysb%d" % ys])
                k.dma("sync", Ys[e_, r0 + rt * 128:r0 + (rt + 1) * 128, :], ysb[ys], r=["ysb%d" % ys])
        k.barrier()

        ar.reset(base4)
        selc6 = [ar.get([128, NE, CT], BF16) for _ in range(2)]
        gsc6 = [ar.get([128, NE, CT], F32) for _ in range(2)]
        gt = [ar.get([128, len(EG), 128], F32) for _ in range(2)]
        yr = [ar.get([128, len(EG), DM], F32) for _ in range(2)]
        yrb = [ar.get([128, len(EG), DM], BF16) for _ in range(2)]
        hh_ = [ar.get([128, DM], F32) for _ in range(2)]
        ob = [ar.get([128, DM], F32) for _ in range(2)]
        junk6 = ar.get([128, DM], BF16)
        st6 = ar.get([128, 2 * NT], F32)
        def load6(c):
            s_ = c % 2
            k.dma("sync", hh_[s_], Hs[c * 128:(c + 1) * 128, :], r=["Hs"], w=["hh%d" % s_])
            for gi_, (e0, ne) in enumerate(EG):
                for el in range(ne):
                    k.dma("sync", yrb[s_][el * CT:(el + 1) * CT, gi_, :], Ys[e0 + el, c * CT:(c + 1) * CT, :], r=["Ys"], w=["yrb%d" % s_])
        def prep6(c):
            s_ = c % 2
            build_sel(c, selc6[s_], gsc6[s_], "c%d" % s_)
            gs2 = gsc6[s_].rearrange("p e j -> p (e j)")
            for gi_, (e0, ne) in enumerate(EG):
                M = ne * CT
                bk = gi_ % 2
                k.tr(PS[0:M, bk, 0:128], gs2[:, e0 * CT:e0 * CT + M], identf[:], r=["c%dgsc" % s_, "identf"], w=["pst%d" % bk])
                k.copy("scalar", gt[s_][0:M, gi_, :], PS[0:M, bk, 0:128], r=["pst%d" % bk], w=["gt%d" % s_])
        load6(0)
        prep6(0)
        for c in range(NT):
            s_ = c % 2
            tok = slice(c * 128, (c + 1) * 128)
            if c + 1 < NT:
                load6(c + 1)
            k.copy("scalar", yr[s_][:, 0:4, :], yrb[s_][:, 0:4, :], r=["yrb%d" % s_], w=["yr%d" % s_])
            k.copy("gpsimd", yr[s_][:, 4:8, :], yrb[s_][:, 4:8, :], r=["yrb%d" % s_], w=["yr%d" % s_])
            for half in range(2):
                for gi_, (e0, ne) in enumerate(EG):
                    M = ne * CT
                    k.mm(PS[:, 2 + half, :], gt[s_][0:M, gi_, :], yr[s_][0:M, gi_, half * 512:(half + 1) * 512], gi_ == 0, gi_ == len(EG) - 1,
                         r=["gt%d" % s_, "yr%d" % s_], w=["psc%d" % half])
            if c + 1 < NT:
                prep6(c + 1)
            for half in range(2):
                hs = slice(half * 512, (half + 1) * 512)
                k.tt("vector", hh_[s_][:, hs], hh_[s_][:, hs], PS[:, 2 + half, :], ALU.add, r=["psc%d" % half, "hh%d" % s_], w=["hh%d" % s_])
            k.act(junk6, hh_[s_], AF.Square, r=["hh%d" % s_], w=["junk6", "ss6_%d" % s_], accum_out=st6[:, 2 * c:2 * c + 1])
            rstd(st6[:, 2 * c + 1:2 * c + 2], st6[:, 2 * c:2 * c + 1], DM, ["ss6_%d" % s_], ["r6_%d" % s_])
            k.stt("vector", ob[s_], hh_[s_], st6[:, 2 * c + 1:2 * c + 2], gfbc[:], ALU.mult, ALU.mult,
                  r=["hh%d" % s_, "r6_%d" % s_, "gfbc"], w=["ob%d" % s_])
            k.dma("sync", out_d[tok, :], ob[s_], r=["ob%d" % s_])
    k.barrier()

    sems = {}
    pools = {}
    import contextlib
    with contextlib.ExitStack() as es:
        for e in ENGS:
            sems[e] = es.enter_context(nc.semaphore("s_" + e))
            pools[e] = [es.enter_context(nc.semaphore("d_%s%d" % (e, i))) for i in range(NPOOL)]
        k.emit(sems, pools)
    return nc


def _consts():
    ident = np.eye(128, dtype=np.float32)
    i = np.arange(128)[:, None]; j = np.arange(128)[None, :]
    triu = (i < j).astype(np.float32)
    mA = (j <= i).astype(np.float32)
    mB = (j >= i).astype(np.float32)
    mask = np.concatenate([mA, mB, mA, mB], axis=1)
    return ident, triu, mask


def _tables():
    def inv_freq(dim):
        return (1.0 / (10000.0 ** (np.arange(0, dim, 2, dtype=np.float32) / np.float32(dim)))).astype(np.float32)
    t = np.arange(S, dtype=np.float32)
    row = np.floor(t / 64).astype(np.float32); col = (t % 64).astype(np.float32)
    f = inv_freq(32)
    angA = np.concatenate([row[:, None] * f, col[:, None] * f], axis=-1).astype(np.float32)
    angB = (t[:, None] * inv_freq(64)).astype(np.float32)
    return np.concatenate([np.cos(angA), np.sin(angA), np.cos(angB), np.sin(angB)], axis=-1).astype(np.float32)


def make_in_maps(inp, cores):
    ident, triu, mask = _consts()
    tabs = _tables()
    f = lambda a: np.ascontiguousarray(a, dtype=np.float32)
    shared = {
        "w_in": f(inp["w_in"][0]), "w_out": f(inp["w_out"][0]),
        "g1": f(inp["norm1_g"][0].reshape(8, 128).T),
        "gab": f(np.concatenate([inp["out_norm_a_g"][0], inp["out_norm_b_g"][0]]).reshape(8, 128).T),
        "g2": f(inp["norm2_g"][0]), "gf": f(inp["final_norm_g"]),
        "gq": f(inp["q_norm_g"][0]), "gk": f(inp["k_norm_g"][0]),
        "rw": f(np.concatenate([inp["router_group_w"][0], inp["router_expert_w"][0]], axis=1)),
        "rb": f(np.concatenate([inp["router_group_b"][0], inp["router_expert_b"][0]])),
        "w_gate": f(inp["w_gate"][0]), "w_up": f(inp["w_up"][0]), "w_down": f(inp["w_down"][0]),
        "tabs": tabs, "c_ident": ident, "c_triu": triu, "c_mask": mask,
        "c_e1024": (np.arange(NE) * CAP).astype(np.float32),
        "c_iota": np.arange(CT).astype(np.float32),
    }
    maps = []
    for c in cores:
        m = dict(shared)
        xb = f(inp["x"][c])
        m["x"] = xb
        maps.append(m)
    return maps


def kernel(**inputs):
    inp = {k_: np.asarray(v) for k_, v in inputs.items()}
    nc = build_nc(debug=False)
    maps = make_in_maps(inp, list(range(8)))
    res = run_bass_kernel_spmd(nc, maps, core_ids=list(range(8)))
    out = np.stack([np.asarray(r["out"]) for r in res.results], axis=0)
    return out.astype(np.float32)
```

```python
import numpy as np
import concourse.bass as bass
import concourse.mybir as mybir
from concourse.bass_utils import run_bass_kernel_spmd

F32 = mybir.dt.float32
BF16 = mybir.dt.bfloat16
I32 = mybir.dt.int32
ALU = mybir.AluOpType
AF = mybir.ActivationFunctionType
AX = mybir.AxisListType

S = 8192
NT = 64
DM = 1024
CAP = 1024
CT = 32
RE = NT * CT
NE = 32
EPS = 1e-6
ENGS = ["tensor", "vector", "scalar", "gpsimd", "sync"]
NPOOL = 16
DEBUG = False


class KB:
    def __init__(self, nc):
        self.nc = nc
        self.ops = {e: [] for e in ENGS}
        self.last_w = {}
        self.readers = {}
        self.ndma = {e: 0 for e in ENGS}
        self.dmas = {e: [] for e in ENGS}

    def op(self, eng, fn, r=(), w=(), dma=False):
        rec = dict(eng=eng, fn=fn, deps=[], sig=False, dma=dma, idx=len(self.ops[eng]))
        deps = []
        for k in r:
            if k in self.last_w:
                deps.append(self.last_w[k])
        for k in w:
            if k in self.last_w:
                deps.append(self.last_w[k])
            deps.extend(self.readers.get(k, ()))
        for k in r:
            self.readers.setdefault(k, []).append(rec)
        for k in w:
            self.last_w[k] = rec
            self.readers[k] = []
        if dma:
            rec["k"] = self.ndma[eng]
            self.ndma[eng] += 1
            self.dmas[eng].append(rec)
        seen = set()
        for d in deps:
            if id(d) in seen or d is rec:
                continue
            seen.add(id(d))
            if d["eng"] == "tensor" and eng == "tensor" and not d["dma"]:
                continue
            d["sig"] = True
            rec["deps"].append(d)
        self.ops[eng].append(rec)
        return rec

    def barrier(self):
        lasts = []
        for e in ENGS:
            for rec in reversed(self.ops[e]):
                if rec["fn"] is not None and not rec["dma"]:
                    rec["sig"] = True
                    lasts.append(rec)
                    break
            lasts.extend(self.dmas[e][-NPOOL:])
        for e in ENGS:
            rec = dict(eng=e, fn=None, deps=list(lasts), sig=False, dma=False, idx=len(self.ops[e]))
            self.ops[e].append(rec)
        self.last_w = {}
        self.readers = {}

    def emit(self, sems, pools):
        nc = self.nc
        for e in ENGS:
            c = 0
            for rec in self.ops[e]:
                if rec["dma"]:
                    k = rec["k"]
                    rec["sem"] = pools[e][k % NPOOL]
                    rec["val"] = 16 * (k // NPOOL + 1)
                elif rec["sig"]:
                    c += 1
                    rec["sem"] = sems[e]
                    rec["val"] = c
        with nc.Block() as block:
            for e in ENGS:
                def mk(e):
                    def body(eng):
                        seen = {}
                        def wait(sem, val):
                            key = id(sem)
                            if seen.get(key, 0) >= val:
                                return
                            seen[key] = val
                            eng.wait_ge(sem, val)
                        for rec in self.ops[e]:
                            for d in rec["deps"]:
                                wait(d["sem"], d["val"])
                            if rec["fn"] is None:
                                continue
                            if rec["dma"]:
                                k = rec["k"]
                                if k >= NPOOL:
                                    wait(pools[e][k % NPOOL], 16 * (k // NPOOL))
                                rec["fn"](eng).then_inc(rec["sem"], 16)
                            else:
                                ins = rec["fn"](eng)
                                if rec["sig"]:
                                    ins.then_inc(rec["sem"], 1)
                    return body
                getattr(block, e)(mk(e))

    def mm(self, out, lhsT, rhs, start, stop, r=(), w=()):
        return self.op("tensor", lambda e: e.matmul(out, lhsT=lhsT, rhs=rhs, start=start, stop=stop), r, w)

    def tr(self, out, in_, ident, r=(), w=()):
        return self.op("tensor", lambda e: e.transpose(out, in_, ident), r, w)

    def act(self, out, in_, func, r=(), w=(), **kw):
        return self.op("scalar", lambda e: e.activation(out=out, in_=in_, func=func, **kw), r, w)

    def tt(self, eng, out, in0, in1, op, r=(), w=()):
        return self.op(eng, lambda e: e.tensor_tensor(out=out, in0=in0, in1=in1, op=op), r, w)

    def ts(self, eng, out, in0, s1, s2, op0, op1=None, r=(), w=()):
        if op1 is None:
            return self.op(eng, lambda e: e.tensor_scalar(out=out, in0=in0, scalar1=s1, scalar2=None, op0=op0), r, w)
        return self.op(eng, lambda e: e.tensor_scalar(out=out, in0=in0, scalar1=s1, scalar2=s2, op0=op0, op1=op1), r, w)

    def stt(self, eng, out, in0, scalar, in1, op0, op1, r=(), w=()):
        return self.op(eng, lambda e: e.scalar_tensor_tensor(out=out, in0=in0, scalar=scalar, in1=in1, op0=op0, op1=op1), r, w)

    def copy(self, eng, out, in_, r=(), w=()):
        if eng == "scalar":
            return self.op(eng, lambda e: e.activation(out=out, in_=in_, func=AF.Copy), r, w)
        return self.op(eng, lambda e: e.tensor_copy(out=out, in_=in_), r, w)

    def red(self, eng, out, in_, op, r=(), w=()):
        return self.op(eng, lambda e: e.tensor_reduce(out=out, in_=in_, axis=AX.X, op=op), r, w)

    def memset(self, eng, ap, val, r=(), w=()):
        return self.op(eng, lambda e: e.memset(ap, val), r, w)

    def dma(self, q, out, in_, r=(), w=()):
        return self.op(q, lambda e: e.dma_start(out=out, in_=in_), r, w, dma=True)

    def scatter(self, out, off, in_, bound, r=(), w=()):
        return self.op("gpsimd", lambda e: e.indirect_dma_start(
            out=out, out_offset=bass.IndirectOffsetOnAxis(ap=off, axis=0), in_=in_, in_offset=None,
            bounds_check=bound, oob_is_err=False), r, w, dma=True)

    def gather(self, out, in_, off, bound, r=(), w=()):
        return self.op("gpsimd", lambda e: e.indirect_dma_start(
            out=out, out_offset=None, in_=in_, in_offset=bass.IndirectOffsetOnAxis(ap=off, axis=0),
            bounds_check=bound, oob_is_err=False), r, w, dma=True)


def build_nc(debug=False, phases=6):
    nc = bass.Bass("TRN2", target_bir_lowering=False)
    dk = "ExternalOutput" if debug else "Internal"

    def din(name, shape, dt=F32):
        return nc.dram_tensor(name, list(shape), dt, kind="ExternalInput")

    x_d = din("x", [S, DM])
    w_in_d = din("w_in", [DM, 2304]); w_out_d = din("w_out", [DM, DM])
    g1_d = din("g1", [128, 8]); gab_d = din("gab", [128, 8]); g2_d = din("g2", [DM]); gf_d = din("gf", [DM])
    gq_d = din("gq", [64]); gk_d = din("gk", [64])
    rw_d = din("rw", [DM, 36]); rb_d = din("rb", [36])
    wg_d = din("w_gate", [NE, DM, 512]); wu_d = din("w_up", [NE, DM, 512]); wd_d = din("w_down", [NE, 512, DM])
    tabs_d = din("tabs", [S, 128])
    cid_d = din("c_ident", [128, 128]); ctri_d = din("c_triu", [128, 128]); cmask_d = din("c_mask", [128, 512])
    ce_d = din("c_e1024", [NE])
    cio_d = din("c_iota", [CT])
    out_d = nc.dram_tensor("out", [S, DM], F32, kind="ExternalOutput")

    QBs = nc.dram_tensor("QBs", [512, S], BF16, kind="Internal")
    KBs = nc.dram_tensor("KBs", [512, S], BF16, kind="Internal")
    VBs = nc.dram_tensor("VBs", [S + 2048, 520], BF16, kind="Internal")
    OT = nc.dram_tensor("OT", [DM, S], BF16, kind=dk)
    Hs = nc.dram_tensor("Hs", [S, DM], F32, kind=dk)
    XN = nc.dram_tensor("XN", [S, DM], BF16, kind="Internal")
    XS = nc.dram_tensor("XS", [NE, RE, DM], BF16, kind="Internal")
    Ys = nc.dram_tensor("Ys", [NE, RE, DM], BF16, kind="Internal")
    LG = nc.dram_tensor("LG", [128, NT * 36], F32, kind=dk)
    DST = nc.dram_tensor("DST", [128, NT * NE], F32, kind=dk)

    def A(name, shape, dt):
        t_ = nc.alloc_sbuf_tensor(name, shape, dt)
        return t_[tuple(slice(None) for _ in shape)]
    identb = A("identb", [128, 128], BF16); identf = A("identf", [128, 128], F32)
    maskAB = A("maskAB", [128, 512], BF16); triU = A("triU", [128, 128], BF16)
    onesb = A("onesb", [128, 128], BF16); onesf = A("onesf", [128, 64], F32)
    g1c = A("g1c", [128, 8], F32); gabc = A("gabc", [128, 8], F32)
    gqk = A("gqk", [128, 12, 64], F32)
    g2bc = A("g2bc", [128, DM], F32); gfbc = A("gfbc", [128, DM], F32)
    rwsb = A("rwsb", [128, 8, 36], F32); rbbc = A("rbbc", [128, 36], F32)
    e1024 = A("e1024", [128, NE], F32)
    iota24 = A("iota24", [128, CT], F32)
    Gfull = A("Gfull", [128, NT, NE], F32)
    ARN = 94400
    arena = A("arena", [128, ARN], BF16)
    PS = nc.alloc_psum_tensor("PS", [128, 8, 512], F32)

    class Arena:
        def __init__(self):
            self.off = 0

        def reset(self, off=0):
            self.off = off

        def get(self, shape, dt):
            n = int(np.prod(shape[1:]))
            nb = n * (2 if dt == BF16 else 4)
            nb = (nb + 63) // 64 * 64
            a = self.off // 2
            self.off += nb
            assert self.off <= ARN * 2, ("arena overflow", self.off)
            v = arena[:, a:a + (n if dt == BF16 else 2 * n)]
            if dt != BF16:
                v = v.bitcast(dt)
            if len(shape) == 3:
                v = v.rearrange("p (a b) -> p a b", a=shape[1])
            elif len(shape) == 4:
                v = v.rearrange("p (a b c) -> p a b c", a=shape[1], b=shape[2])
            return v

    ar = Arena()
    k = KB(nc)
    epsc = A("epsc", [128, 1], F32)
    k.memset("vector", epsc[:], EPS, w=["epsc"])

    def rstd(out, in_, n, rk, wk):
        k.act(out, in_, AF.Sqrt, r=rk + ["epsc"], w=wk, scale=1.0 / n, bias=epsc[:, 0:1])
        k.op("vector", lambda e: e.reciprocal(out=out, in_=out), r=wk, w=wk)

    k.dma("gpsimd", identb[:], cid_d[:, :], w=["identb"])
    k.dma("sync", identf[:], cid_d[:, :], w=["identf"])
    k.dma("gpsimd", maskAB[:], cmask_d[:, :], w=["maskAB"])
    k.dma("gpsimd", triU[:], ctri_d[:, :], w=["triU"])
    k.memset("vector", onesb[:], 1.0, w=["onesb"])
    k.memset("vector", onesf[:], 1.0, w=["onesf"])
    k.dma("sync", g1c[:], g1_d[:, :], w=["g1c"])
    k.dma("sync", gabc[:], gab_d[:, :], w=["gabc"])
    for h in range(12):
        src = gq_d if h < 8 else gk_d
        k.dma("sync", gqk[:, h, :], src[:].partition_broadcast(128), w=["gqk"])
    k.ts("vector", gqk[:, 0:8, :], gqk[:, 0:8, :], 0.125, None, ALU.mult, r=["gqk"], w=["gqk"])
    k.dma("sync", g2bc[:], g2_d[:].partition_broadcast(128), w=["g2bc"])
    k.dma("sync", gfbc[:], gf_d[:].partition_broadcast(128), w=["gfbc"])
    k.dma("sync", rwsb[:], rw_d[:, :].rearrange("(k p) n -> p k n", p=128), w=["rwsb"])
    k.dma("sync", rbbc[:], rb_d[:].partition_broadcast(128), w=["rbbc"])
    k.dma("sync", e1024[:], ce_d[:].partition_broadcast(128), w=["e1024"])
    k.dma("sync", iota24[:], cio_d[:].partition_broadcast(128), w=["iota24"])

    win = ar.get([128, 8, 2432], BF16)
    QAT = ar.get([128, 4, S], BF16)
    KAT = ar.get([128, 2, S], BF16)
    VA = ar.get([128, NT, 130], BF16)
    base12 = ar.off

    wst = ar.get([128, 2304], F32)
    zt = ar.get([128, 8, 520], BF16)
    for kc in range(8):
        k.dma("sync", wst, w_in_d[kc * 128:(kc + 1) * 128, :], w=["wst"])
        gcol = g1c[:, kc:kc + 1]
        k.ts("vector", win[:, kc, 0:512], wst[:, 0:512], gcol, None, ALU.mult, r=["wst", "g1c"], w=["win"])
        k.ts("vector", win[:, kc, 512:2048], wst[:, 768:2304], gcol, None, ALU.mult, r=["wst", "g1c"], w=["win"])
        k.ts("gpsimd", win[:, kc, 2048:2304].rearrange("p (a b c) -> p a b c", a=2, b=2),
             wst[:, 512:640].rearrange("p (a b c) -> p a b c", a=2, b=1).broadcast_to([128, 2, 2, 64]),
             gcol, None, ALU.mult, r=["wst", "g1c"], w=["win"])
        k.ts("gpsimd", win[:, kc, 2304:2432], wst[:, 640:768], gcol, None, ALU.mult, r=["wst", "g1c"], w=["win"])
    k.memset("gpsimd", zt, 0.0, w=["zt"])
    k.dma("sync", VBs[0:1024, :].rearrange("(n p) c -> p n c", p=128), zt, r=["zt"])
    k.dma("sync", VBs[S + 1024:S + 2048, :].rearrange("(n p) c -> p n c", p=128), zt, r=["zt"])
    k.memset("gpsimd", VA.rearrange("p n (k d) -> p n k d", k=2)[:, :, :, 64:65], 1.0, w=["VA"])
    k.barrier()

    ar.reset(base12)
    xTb_ = ar.get([128, 8, 128], BF16); xTb = [xTb_, xTb_]
    xt = ar.get([128, DM], F32)
    junk = ar.get([128, DM], BF16)
    tab = [ar.get([128, 128], F32) for _ in range(2)]
    qk = ar.get([128, 12, 64], F32)
    sq = ar.get([128, 12, 64], F32)
    qkr = ar.get([128, 12, 64], BF16)
    t1 = ar.get([128, 12, 32], F32); t2 = ar.get([128, 12, 32], F32)
    qkb = ar.get([128, 16, 64], F32)
    qkbr = ar.get([128, 16, 64], BF16)
    u1 = ar.get([128, 16, 32], F32); u2 = ar.get([128, 16, 32], F32)
    vbst = [ar.get([128, 8, 65], BF16) for _ in range(2)]
    stT_ = ar.get([128, 8, 128], BF16); stT = [stT_, stT_]
    st1 = ar.get([128, 32], F32)
    for s_ in range(2):
        k.memset("gpsimd", vbst[s_][:, :, 64:65], 1.0, w=["vbst%d" % s_])

    def rotary(eng, src, dst, ta, tb, cs, sn, nh, kr, kw, pre):
        C = cs.unsqueeze(1).broadcast_to([128, nh, 32]); Sn = sn.unsqueeze(1).broadcast_to([128, nh, 32])
        x1 = src[:, :, 0:32]; x2 = src[:, :, 32:64]
        k.tt(eng, ta, x1, C, ALU.mult, r=kr, w=[pre + "a"])
        k.tt(eng, tb, x2, Sn, ALU.mult, r=kr, w=[pre + "b"])
        k.tt(eng, dst[:, :, 0:32], ta, tb, ALU.subtract, r=[pre + "a", pre + "b"], w=kw)
        k.tt(eng, ta, x2, C, ALU.mult, r=kr + kw, w=[pre + "a"])
        k.tt(eng, tb, x1, Sn, ALU.mult, r=kr + kw, w=[pre + "b"])
        k.tt(eng, dst[:, :, 32:64], ta, tb, ALU.add, r=[pre + "a", pre + "b"], w=kw)

    def stageA(t):
        s_ = t % 2
        tok = slice(t * 128, (t + 1) * 128)
        rs = st1[:, 1:2]
        k.dma("sync", xt, x_d[tok, :], w=["xt"])
        k.dma("sync", tab[s_], tabs_d[tok, :], w=["tab%d" % s_])
        sqj = sq.rearrange("p h d -> p (h d)").bitcast(BF16)[:, 0:DM]
        k.act(sqj, xt, AF.Square, r=["xt"], w=["sq", "ss"], accum_out=st1[:, 0:1])
        k.copy("scalar", junk, xt, r=["xt"], w=["junk"])
        pX = PS[:, 7, :].bitcast(BF16).rearrange("p (c t) -> p c t", c=8)
        for kc in range(8):
            k.tr(pX[:, kc, :], junk[:, kc * 128:(kc + 1) * 128], identb[:], r=["junk", "identb"], w=["ps7"])
        k.copy("vector", xTb[s_], pX, r=["ps7"], w=["xTb"])
        rstd(st1[:, 1:2], st1[:, 0:1], DM, ["ss"], ["rstd1"])
        for b in range(5):
            n0, n1 = b * 512, min((b + 1) * 512, 2432)
            for kc in range(8):
                k.mm(PS[:, b, 0:n1 - n0], xTb[s_][:, kc, :], win[:, kc, n0:n1], kc == 0, kc == 7,
                     r=["xTb", "win"], w=["ps%d" % b])

    def stageB1(t):
        s_ = t % 2
        tok = slice(t * 128, (t + 1) * 128)
        rs = st1[:, 1:2]
        k.act(qk[:, 0:8, :], PS[:, 0, :].rearrange("p (h d) -> p h d", h=8), AF.Copy, r=["ps0", "rstd1"], w=["qk"], scale=rs)
        k.act(qk[:, 8:12, :], PS[:, 4, 0:256].rearrange("p (h d) -> p h d", h=4), AF.Copy, r=["ps4", "rstd1"], w=["qk"], scale=rs)
        k.act(VA[:, t, :].rearrange("p (k d) -> p k d", k=2)[:, :, 0:64], PS[:, 4, 256:384].rearrange("p (k d) -> p k d", k=2),
              AF.Copy, r=["ps4", "rstd1"], w=["VA"], scale=rs)
        k.act(qkb[:, 0:8, :], PS[:, 1, :].rearrange("p (h d) -> p h d", h=8), AF.Copy, r=["ps1", "rstd1"], w=["qkb"], scale=rs)
        k.act(qkb[:, 8:16, :], PS[:, 2, :].rearrange("p (h d) -> p h d", h=8), AF.Copy, r=["ps2", "rstd1"], w=["qkb"], scale=rs)
        k.act(vbst[s_][:, :, 0:64], PS[:, 3, :].rearrange("p (h d) -> p h d", h=8), AF.Copy,
              r=["ps3", "rstd1"], w=["vbst%d" % s_], scale=rs)

    def stageB2(t):
        s_ = t % 2
        tok = slice(t * 128, (t + 1) * 128)
        rs = st1[:, 1:2]
        k.tt("vector", sq, qk, qk, ALU.mult, r=["qk"], w=["sq"])
        k.red("vector", st1[:, 2:14], sq, ALU.add, r=["sq"], w=["ssq"])
        rstd(st1[:, 14:26], st1[:, 2:14], 64, ["ssq"], ["rq"])
        k.tt("vector", qk, qk, st1[:, 14:26].unsqueeze(2).broadcast_to([128, 12, 64]), ALU.mult, r=["qk", "rq"], w=["qk"])
        k.tt("vector", qk, qk, gqk, ALU.mult, r=["qk", "gqk"], w=["qk"])
        rotary("vector", qk, qkr, t1, t2, tab[s_][:, 0:32], tab[s_][:, 32:64], 12, ["qk", "tab%d" % s_], ["qkr"], "t")
        k.ts("gpsimd", qkb[:, 0:8, :], qkb[:, 0:8, :], 0.125, None, ALU.mult, r=["qkb"], w=["qkb"])
        rotary("gpsimd", qkb, qkbr, u1, u2, tab[s_][:, 64:96], tab[s_][:, 96:128], 16, ["qkb", "tab%d" % s_], ["qkbr"], "u")
        k.dma("sync", VBs[1024 + t * 128:1024 + (t + 1) * 128, :], vbst[s_].rearrange("p h d -> p (h d)"),
              r=["vbst%d" % s_])
        pA = PS[:, 5, 0:384].bitcast(BF16).rearrange("p (c t) -> p c t", c=6)
        qkr2 = qkr.rearrange("p h d -> p (h d)")
        for c in range(6):
            k.tr(pA[:, c, :], qkr2[:, c * 128:(c + 1) * 128], identb[:], r=["qkr", "identb"], w=["ps5"])
        k.copy("vector", QAT[:, :, tok], pA[:, 0:4, :], r=["ps5"], w=["QAT"])
        k.copy("vector", KAT[:, :, tok], pA[:, 4:6, :], r=["ps5"], w=["KAT"])
        pB = PS[:, 6, :].bitcast(BF16).rearrange("p (c t) -> p c t", c=8)
        qkbr2 = qkbr.rearrange("p h d -> p (h d)")
        for c in range(8):
            k.tr(pB[:, c, :], qkbr2[:, c * 128:(c + 1) * 128], identb[:], r=["qkbr", "identb"], w=["ps6"])
        k.copy("scalar", stT[s_], pB, r=["ps6"], w=["stT"])
        k.dma("sync", QBs[:, tok].rearrange("(c p) t -> p c t", p=128), stT[s_][:, 0:4, :], r=["stT"])
        k.dma("sync", KBs[:, tok].rearrange("(c p) t -> p c t", p=128), stT[s_][:, 4:8, :], r=["stT"])

    if phases >= 1:
        stageA(0)
        for t in range(NT):
            stageB1(t)
            if t + 1 < NT:
                stageA(t + 1)
            stageB2(t)
    k.barrier()

    def epilogue(src65, srckey, osb, rden, obf, slot, row0, col0, ncol):
        k.copy("vector", osb[0:65, 0:ncol], src65, r=[srckey], w=["osb%d" % slot])
        k.op("vector", lambda e: e.reciprocal(out=rden[64:65, 0:ncol], in_=osb[64:65, 0:ncol]), r=["osb%d" % slot], w=["rden%d" % slot])
        k.mm(PS[0:64, 7, 0:ncol], onesf[64:65, 0:64], rden[64:65, 0:ncol], True, True, r=["rden%d" % slot, "onesf"], w=["ps7"])
        k.tt("vector", obf[0:64, 0:ncol], osb[0:64, 0:ncol], PS[0:64, 7, 0:ncol], ALU.mult, r=["osb%d" % slot, "ps7"], w=["obf%d" % slot])
        k.dma("sync", OT[row0:row0 + 64, col0:col0 + ncol], obf[0:64, 0:ncol], r=["obf%d" % slot])

    ar.reset(base12)
    Pb = [ar.get([128, 512], BF16) for _ in range(3)]
    osb = [ar.get([128, 512], F32) for _ in range(2)]
    rden = [ar.get([128, 512], F32) for _ in range(2)]
    obf = [ar.get([128, 512], BF16) for _ in range(2)]
    VA4 = VA.rearrange("p n (k d) -> p n k d", k=2)
    Pb4 = [[Pb[0], Pb[1]], [Pb[2], ar.get([128, 512], BF16)]]
    for pair in range(4 if phases >= 2 else 0):
        kv = pair // 2
        for qb in range(16):
            qs = slice(qb * 512, (qb + 1) * 512)

            def qk_mm(kc):
                for hh in range(2):
                    b0 = 64 * hh
                    sb = 2 * hh + kc % 2
                    k.mm(PS[:, sb, :], KAT[b0:b0 + 64, kv, kc * 128:(kc + 1) * 128], QAT[b0:b0 + 64, pair, qs], True, True,
                         r=["QAT", "KAT"], w=["psS%d" % sb])
            qk_mm(0)
            for kc in range(NT):
                if kc + 1 < NT:
                    qk_mm(kc + 1)
                for hh in range(2):
                    sb = 2 * hh + kc % 2
                    k.act(Pb4[hh][kc % 2], PS[:, sb, :], AF.Exp, r=["psS%d" % sb], w=["Pb%d" % sb])
                for hh in range(2):
                    sb = 2 * hh + kc % 2
                    k.mm(PS[0:65, 4 + hh, :], VA4[:, kc, kv, :], Pb4[hh][kc % 2], kc == 0, kc == NT - 1, r=["VA", "Pb%d" % sb], w=["psO%d" % hh])
            for hh in range(2):
                epilogue(PS[0:65, 4 + hh, :], "psO%d" % hh, osb[hh], rden[hh], obf[hh], hh, (2 * pair + hh) * 64, qb * 512, 512)
    k.barrier()

    ar.reset(0)
    KBT = ar.get([128, S + 2048], BF16)
    QBT = ar.get([128, S], BF16)
    acc = ar.get([128, S], F32)
    vt = [ar.get([128, 65, 130], BF16) for _ in range(2)]
    Pe = [ar.get([128, 512], BF16) for _ in range(3)]
    Pm = [ar.get([128, 512], BF16) for _ in range(3)]
    osb3 = ar.get([128, 512], F32); rden3 = ar.get([128, 512], F32); obf3 = ar.get([128, 512], BF16)
    if phases >= 3:
        k.memset("gpsimd", KBT[:, 0:1024], 0.0, w=["KBT"])
        k.memset("gpsimd", KBT[:, S + 1024:S + 2048], 0.0, w=["KBT"])
    vi = 0
    gi = 0
    for pair in range(4 if phases >= 3 else 0):
        k.dma("sync", KBT[:, 1024:1024 + S], KBs[pair * 128:(pair + 1) * 128, :], w=["KBT"])
        k.dma("sync", QBT[:, :], QBs[pair * 128:(pair + 1) * 128, :], w=["QBT"])
        for hh in range(2):
            h = pair * 2 + hh
            b0 = 64 * hh
            k.memset("gpsimd", acc[0:65, :], 0.0, w=["acc"])
            for Dl in (1, 4, 16):
                L = S // Dl
                nq = L // 128
                for o in range(Dl):
                    vs = vi % 2
                    vi += 1
                    start = 1024 + o - 64 * Dl
                    nch = nq + 1
                    vsrc = VBs[start:start + Dl * (128 * nch - 1) + 1:Dl, pair * 130:(pair + 1) * 130].rearrange("(n p) c -> p n c", p=128)
                    k.dma("sync", vt[vs][:, 0:nch, :], vsrc, r=["VBs"], w=["vt%d" % vs])
                    for c0 in range(0, nq, 2):
                        g = gi % 3
                        gi += 1
                        psS = PS[:, g, :]
                        psO = PS[0:65, 3 + g, 0:256]
                        for cc in range(2):
                            c = c0 + cc
                            q0 = o + Dl * 128 * c
                            qsl = QBT[b0:b0 + 64, q0:q0 + Dl * 127 + 1:Dl]
                            for ty in range(2):
                                n = c + ty
                                k0 = 1024 + o + Dl * (128 * n - 64)
                                ksl = KBT[b0:b0 + 64, k0:k0 + Dl * 127 + 1:Dl]
                                j = cc * 2 + ty
                                k.mm(psS[:, j * 128:(j + 1) * 128], ksl, qsl, True, True, r=["KBT", "QBT"], w=["psS%d" % g])
                        k.act(Pe[g], psS, AF.Exp, r=["psS%d" % g], w=["Pe%d" % g])
                        k.tt("vector", Pm[g], Pe[g], maskAB[:], ALU.mult, r=["Pe%d" % g, "maskAB"], w=["Pm%d" % g])
                        for cc in range(2):
                            c = c0 + cc
                            for ty in range(2):
                                n = c + ty
                                j = cc * 2 + ty
                                k.mm(psO[:, cc * 128:(cc + 1) * 128], vt[vs][:, n, hh * 65:(hh + 1) * 65], Pm[g][:, j * 128:(j + 1) * 128],
                                     ty == 0, ty == 1, r=["vt%d" % vs, "Pm%d" % g], w=["psO%d" % g])
                        q0 = o + Dl * 128 * c0
                        av = acc[0:65, q0:q0 + Dl * 255 + 1:Dl]
                        k.tt("vector", av, psO, av, ALU.add, r=["psO%d" % g, "acc"], w=["acc"])
            for qb in range(16):
                epilogue(acc[0:65, qb * 512:(qb + 1) * 512], "acc", osb3, rden3, obf3, 0, 512 + h * 64, qb * 512, 512)
    k.barrier()

    ar.reset(0)
    wout = ar.get([128, 8, DM], BF16)
    logits = ar.get([128, NT, 36], F32)
    wth = ar.get([128, NT, NE], F32)
    mselp = ar.get([128, NT, NE], BF16)
    base4 = ar.off
    wst2 = ar.get([128, DM], F32)
    for jc in range(8 if phases >= 4 else 0):
        k.dma("sync", wst2, w_out_d[jc * 128:(jc + 1) * 128, :], w=["wst2"])
        k.ts("vector", wout[:, jc, :], wst2, gabc[:, jc:jc + 1], None, ALU.mult, r=["wst2", "gabc"], w=["wout"])
    ot = [ar.get([128, 8, 512], BF16) for _ in range(2)]
    osq = ar.get([128, 8, 128], F32)
    xt4 = ar.get([128, DM], F32)
    hbuf = [ar.get([128, DM], F32) for _ in range(2)]
    xn = ar.get([128, DM], F32)
    xnb = [ar.get([128, DM], BF16) for _ in range(2)]
    xnT = ar.get([128, 8, 128], F32)
    junk4 = ar.get([128, DM], BF16)
    st4 = ar.get([128, 16], F32)
    for t in range(NT if phases >= 4 else 0):
        s_ = t % 2
        tok = slice(t * 128, (t + 1) * 128)
        blk, sub = t // 4, t % 4
        bs = blk % 2
        if sub == 0:
            k.dma("sync", ot[bs], OT[:, blk * 512:(blk + 1) * 512].rearrange("(c p) t -> p c t", p=128), r=["OT"], w=["ot%d" % bs])
        k.dma("sync", xt4, x_d[tok, :], w=["xt4"])
        otl = ot[bs][:, :, sub * 128:(sub + 1) * 128]
        k.tt("gpsimd", osq, otl, otl, ALU.mult, r=["ot%d" % bs], w=["osq"])
        for half in range(2):
            for jc in range(4):
                k.mm(PS[:, 6, half:half + 1], osq[:, half * 4 + jc, :], onesf[:, 0:1], jc == 0, jc == 3, r=["osq", "onesf"], w=["ps6"])
        rstd(st4[:, 2:4], PS[:, 6, 0:2], 512, ["ps6"], ["rab"])
        for half in range(2):
            for part in range(2):
                for jc in range(4):
                    k.mm(PS[:, half * 2 + part, :], otl[:, part * 4 + jc, :], wout[:, part * 4 + jc, half * 512:(half + 1) * 512],
                         jc == 0, jc == 3, r=["ot%d" % bs, "wout"], w=["ps%d" % (half * 2 + part)])
        hb = hbuf[s_]
        for half in range(2):
            hs = slice(half * 512, (half + 1) * 512)
            k.stt("vector", hb[:, hs], PS[:, half * 2, :], st4[:, 2:3], xt4[:, hs], ALU.mult, ALU.add,
                  r=["ps%d" % (half * 2), "rab", "xt4"], w=["hb%d" % s_])
            k.stt("vector", hb[:, hs], PS[:, half * 2 + 1, :], st4[:, 3:4], hb[:, hs], ALU.mult, ALU.add,
                  r=["ps%d" % (half * 2 + 1), "rab", "hb%d" % s_], w=["hb%d" % s_])
        k.dma("sync", Hs[tok, :], hb, r=["hb%d" % s_])
        k.act(junk4, hb, AF.Square, r=["hb%d" % s_], w=["junk4", "ss2"], accum_out=st4[:, 4:5])
        rstd(st4[:, 5:6], st4[:, 4:5], DM, ["ss2"], ["r2"])
        k.stt("vector", xn, hb, st4[:, 5:6], g2bc[:], ALU.mult, ALU.mult, r=["hb%d" % s_, "r2", "g2bc"], w=["xn"])
        pT = PS[:, 4:6, :].rearrange("p a (c t) -> p (a c) t", c=4)
        for kc in range(8):
            k.tr(pT[:, kc, :], xn[:, kc * 128:(kc + 1) * 128], identf[:], r=["xn", "identf"], w=["ps45"])
        k.copy("scalar", xnT, pT, r=["ps45"], w=["xnT"])
        k.copy("gpsimd", xnb[s_], xn, r=["xn"], w=["xnb%d" % s_])
        k.dma("sync", XN[tok, :], xnb[s_], r=["xnb%d" % s_])
        for kc in range(8):
            k.mm(PS[:, 7, 0:36], xnT[:, kc, :], rwsb[:, kc, :], kc == 0, kc == 7, r=["xnT", "rwsb"], w=["ps7"])
        k.tt("vector", logits[:, t, :], PS[:, 7, 0:36], rbbc[:], ALU.add, r=["ps7", "rbbc"], w=["logits"])
    if debug and phases >= 4:
        k.dma("sync", LG[:, :], logits.rearrange("p t n -> p (t n)"), r=["logits"])
    k.barrier()

    ar.reset(base4)
    BIG = 1.0e30
    if phases >= 5:
        V = "vector"
        gl = logits[:, :, 0:4]
        el = logits[:, :, 4:36]
        gm = ar.get([128, NT], F32); ge = ar.get([128, NT, 4], F32); gs = ar.get([128, NT], F32)
        ohg = ar.get([128, NT, 4], F32); pen = ar.get([128, NT, 4], F32)
        em = ar.get([128, NT, 32], F32); em2 = ar.get([128, NT, 32], F32)
        m1 = ar.get([128, NT], F32); m2 = ar.get([128, NT], F32)
        oh1 = ar.get([128, NT, 32], F32); oh2 = ar.get([128, NT, 32], F32)
        p1 = ar.get([128, NT], F32); ga = ar.get([128, NT], F32); gb = ar.get([128, NT], F32)

        def b4(a):
            return a.unsqueeze(2).broadcast_to([128, NT, 4])

        def b32(a):
            return a.unsqueeze(2).broadcast_to([128, NT, 32])
        k.red(V, gm, gl, ALU.max, r=["logits"], w=["gm"])
        k.tt(V, ge, gl, b4(gm), ALU.subtract, r=["logits", "gm"], w=["ge"])
        k.tt(V, ohg, gl, b4(gm), ALU.is_equal, r=["logits", "gm"], w=["ohg"])
        k.act(ge, ge, AF.Exp, r=["ge"], w=["ge"])
        k.red(V, gs, ge, ALU.add, r=["ge"], w=["gs"])
        k.op(V, lambda e: e.reciprocal(out=gs, in_=gs), r=["gs"], w=["gs"])
        k.ts(V, pen, ohg, BIG, -BIG, ALU.mult, ALU.add, r=["ohg"], w=["pen"])
        k.tt(V, em.rearrange("p t (g e) -> p t g e", g=4), el.rearrange("p t (g e) -> p t g e", g=4),
             pen.unsqueeze(3).broadcast_to([128, NT, 4, 8]), ALU.add, r=["logits", "pen"], w=["em"])
        k.red(V, m1, em, ALU.max, r=["em"], w=["m1"])
        k.tt(V, oh1, em, b32(m1), ALU.is_equal, r=["em", "m1"], w=["oh1"])
        k.stt(V, em2, oh1, -BIG, em, ALU.mult, ALU.add, r=["oh1", "em"], w=["em2"])
        k.red(V, m2, em2, ALU.max, r=["em2"], w=["m2"])
        k.tt(V, oh2, em2, b32(m2), ALU.is_equal, r=["em2", "m2"], w=["oh2"])
        k.tt(V, p1, m2, m1, ALU.subtract, r=["m1", "m2"], w=["p1"])
        k.act(p1, p1, AF.Exp, r=["p1"], w=["p1"])
        k.ts(V, p1, p1, 1.0, None, ALU.add, r=["p1"], w=["p1"])
        k.op(V, lambda e: e.reciprocal(out=p1, in_=p1), r=["p1"], w=["p1"])
        k.tt(V, mselp, oh1, oh2, ALU.add, r=["oh1", "oh2"], w=["mselp"])
        msel2 = mselp.rearrange("p t e -> p (t e)")
        wth2 = wth.rearrange("p t e -> p (t e)")
        for q in range(4):
            k.mm(PS[:, q, :], triU[:], msel2[:, q * 512:(q + 1) * 512], True, True, r=["mselp", "triU"], w=["ps%d" % q])
            k.copy("scalar", wth2[:, q * 512:(q + 1) * 512], PS[:, q, :], r=["ps%d" % q], w=["wth"])
        k.tt(V, ga, gs, p1, ALU.mult, r=["gs", "p1"], w=["ga"])
        k.tt(V, gb, gs, ga, ALU.subtract, r=["gs", "ga"], w=["gb"])
        k.tt(V, oh1, oh1, b32(ga), ALU.mult, r=["oh1", "ga"], w=["oh1"])
        k.tt(V, oh2, oh2, b32(gb), ALU.mult, r=["oh2", "gb"], w=["oh2"])
        k.tt(V, Gfull, oh1, oh2, ALU.add, r=["oh1", "oh2"], w=["Gfull"])
        if debug:
            k.dma("sync", DST[:, :], Gfull.rearrange("p t e -> p (t e)"), r=["Gfull"])
    k.barrier()

    EG = [(g * 4, 4) for g in range(8)]
    V = "vector"

    def build_sel(c, selc, gsc=None, pre=""):
        w3 = wth[:, c, :].unsqueeze(2).broadcast_to([128, NE, CT])
        io = iota24[:, :].unsqueeze(1).broadcast_to([128, NE, CT])
        k.tt("vector", selc, w3, io, ALU.is_equal, r=["wth", "iota24"], w=[pre + "selc"])
        k.tt("gpsimd", selc, selc, mselp[:, c, :].unsqueeze(2).broadcast_to([128, NE, CT]), ALU.mult, r=[pre + "selc", "mselp"], w=[pre + "selc"])
        if gsc is not None:
            k.tt("gpsimd", gsc, selc, Gfull[:, c, :].unsqueeze(2).broadcast_to([128, NE, CT]), ALU.mult, r=[pre + "selc", "Gfull"], w=[pre + "gsc"])

    if phases >= 5:
        ar.reset(base4)
        xg = [ar.get([128, DM], BF16) for _ in range(2)]
        selc_ = [ar.get([128, NE, CT], BF16) for _ in range(2)]
        xrow = [ar.get([128, DM], BF16) for _ in range(4)]
        xi = 0
        k.dma("sync", xg[0], XN[0:128, :], r=["XN"], w=["xg0"])
        for c in range(NT):
            s_ = c % 2
            if c + 1 < NT:
                k.dma("sync", xg[1 - s_], XN[(c + 1) * 128:(c + 2) * 128, :], r=["XN"], w=["xg%d" % (1 - s_)])
            build_sel(c, selc_[s_], None, "d%d" % s_)
            sel2 = selc_[s_].rearrange("p e j -> p (e j)")
            for (e0, ne) in EG:
                M = ne * CT
                xr = xi % 4
                xi += 1
                for half in range(2):
                    bk = (xr % 2) * 2 + half
                    k.mm(PS[0:M, bk, :], sel2[:, e0 * CT:e0 * CT + M], xg[s_][:, half * 512:(half + 1) * 512], True, True,
                         r=["d%dselc" % s_, "xg%d" % s_], w=["psd%d" % bk])
                    k.copy("scalar" if half == 0 else "vector", xrow[xr][0:M, half * 512:(half + 1) * 512], PS[0:M, bk, :],
                           r=["psd%d" % bk], w=["xrow%d" % xr])
                for el in range(ne):
                    k.dma("sync", XS[e0 + el, c * CT:(c + 1) * CT, :], xrow[xr][el * CT:(el + 1) * CT, :], r=["xrow%d" % xr])
        k.barrier()

        ar.reset(base4)
        wg = [ar.get([128, 8, 512], BF16) for _ in range(2)]
        wu = [ar.get([128, 8, 512], BF16) for _ in range(2)]
        wd = [ar.get([128, 4, DM], BF16) for _ in range(2)]
        xs = [ar.get([128, 4, DM], BF16) for _ in range(2)]
        xsT = ar.get([128, 8, 512], BF16)
        sg = [ar.get([128, 512], F32) for _ in range(2)]
        hT = ar.get([128, 4, 512], BF16)
        ysb = [ar.get([128, DM], BF16) for _ in range(2)]
        yi = 0
        NBLK = RE // 512
        blocks = [(e_, b_) for e_ in range(NE) for b_ in range(NBLK)]

        def load_w(e_):
            ws = e_ % 2
            k.dma("gpsimd", wg[ws], wg_d[e_].rearrange("(k p) f -> p k f", p=128), w=["wg%d" % ws])
            k.dma("gpsimd", wu[ws], wu_d[e_].rearrange("(k p) f -> p k f", p=128), w=["wu%d" % ws])
            k.dma("gpsimd", wd[ws], wd_d[e_].rearrange("(k p) f -> p k f", p=128), w=["wd%d" % ws])

        def load_x(i):
            e_, b_ = blocks[i]
            k.dma("sync", xs[i % 2], XS[e_, b_ * 512:(b_ + 1) * 512, :].rearrange("(a p) d -> p a d", p=128), r=["XS"], w=["xs%d" % (i % 2)])
        load_w(0)
        load_x(0)
        for i, (e_, blk) in enumerate(blocks):
            ws = e_ % 2
            xb = i % 2
            r0 = blk * 512
            if blk == 0 and e_ + 1 < NE:
                load_w(e_ + 1)
            if i + 1 < len(blocks):
                load_x(i + 1)
            for kc in range(8):
                tb = kc % 2
                pT = PS[:, tb, 0:256].bitcast(BF16).rearrange("p (a t) -> p a t", a=4)
                for a in range(4):
                    k.tr(pT[:, a, :], xs[xb][:, a, kc * 128:(kc + 1) * 128], identb[:], r=["xs%d" % xb, "identb"], w=["pst%d" % tb])
                k.copy("vector" if kc % 2 == 0 else "scalar", xsT[:, kc, :], PS[:, tb, 0:256].bitcast(BF16), r=["pst%d" % tb], w=["xsT"])
            for fc in range(4):
                pb = fc % 2
                for kc in range(8):
                    k.mm(PS[:, 2 + pb, :], wg[ws][:, kc, fc * 128:(fc + 1) * 128], xsT[:, kc, :], kc == 0, kc == 7,
                         r=["wg%d" % ws, "xsT"], w=["psg%d" % pb])
                for kc in range(8):
                    k.mm(PS[:, 4 + pb, :], wu[ws][:, kc, fc * 128:(fc + 1) * 128], xsT[:, kc, :], kc == 0, kc == 7,
                         r=["wu%d" % ws, "xsT"], w=["psu%d" % pb])
                k.act(sg[pb], PS[:, 2 + pb, :], AF.Silu, r=["psg%d" % pb], w=["sg%d" % pb])
                k.tt("vector", hT[:, fc, :], sg[pb], PS[:, 4 + pb, :], ALU.mult, r=["sg%d" % pb, "psu%d" % pb], w=["hT"])
            for rt in range(4):
                ys = yi % 2
                yi += 1
                for dh in range(2):
                    for fc in range(4):
                        k.mm(PS[:, 6 + dh, :], hT[:, fc, rt * 128:(rt + 1) * 128], wd[ws][:, fc, dh * 512:(dh + 1) * 512], fc == 0, fc == 3,
                             r=["hT", "wd%d" % ws], w=["psy%d" % dh])
                k.copy("scalar", ysb[ys][:, 0:512], PS[:, 6, :], r=["psy0"], w=["ysb%d" % ys])
                k.copy("vector", ysb[ys][:, 512:1024], PS[:, 7, :], r=["psy1"], w=["ysb%d" % ys])
                k.dma("sync", Ys[e_, r0 + rt * 128:r0 + (rt + 1) * 128, :], ysb[ys], r=["ysb%d" % ys])
        k.barrier()

        ar.reset(base4)
        selc6 = [ar.get([128, NE, CT], BF16) for _ in range(2)]
        gsc6 = [ar.get([128, NE, CT], F32) for _ in range(2)]
        gt = [ar.get([128, len(EG), 128], F32) for _ in range(2)]
        yr = [ar.get([128, len(EG), DM], F32) for _ in range(2)]
        yrb = [ar.get([128, len(EG), DM], BF16) for _ in range(2)]
        hh_ = [ar.get([128, DM], F32) for _ in range(2)]
        ob = [ar.get([128, DM], F32) for _ in range(2)]
        junk6 = ar.get([128, DM], BF16)
        st6 = ar.get([128, 2 * NT], F32)
        def load6(c):
            s_ = c % 2
            k.dma("sync", hh_[s_], Hs[c * 128:(c + 1) * 128, :], r=["Hs"], w=["hh%d" % s_])
            for gi_, (e0, ne) in enumerate(EG):
                for el in range(ne):
                    k.dma("sync", yrb[s_][el * CT:(el + 1) * CT, gi_, :], Ys[e0 + el, c * CT:(c + 1) * CT, :], r=["Ys"], w=["yrb%d" % s_])
        def prep6(c):
            s_ = c % 2
            build_sel(c, selc6[s_], gsc6[s_], "c%d" % s_)
            gs2 = gsc6[s_].rearrange("p e j -> p (e j)")
            for gi_, (e0, ne) in enumerate(EG):
                M = ne * CT
                bk = gi_ % 2
                k.tr(PS[0:M, bk, 0:128], gs2[:, e0 * CT:e0 * CT + M], identf[:], r=["c%dgsc" % s_, "identf"], w=["pst%d" % bk])
                k.copy("scalar", gt[s_][0:M, gi_, :], PS[0:M, bk, 0:128], r=["pst%d" % bk], w=["gt%d" % s_])
        load6(0)
        prep6(0)
        for c in range(NT):
            s_ = c % 2
            tok = slice(c * 128, (c + 1) * 128)
            if c + 1 < NT:
                load6(c + 1)
            k.copy("scalar", yr[s_][:, 0:4, :], yrb[s_][:, 0:4, :], r=["yrb%d" % s_], w=["yr%d" % s_])
            k.copy("gpsimd", yr[s_][:, 4:8, :], yrb[s_][:, 4:8, :], r=["yrb%d" % s_], w=["yr%d" % s_])
            for half in range(2):
                for gi_, (e0, ne) in enumerate(EG):
                    M = ne * CT
                    k.mm(PS[:, 2 + half, :], gt[s_][0:M, gi_, :], yr[s_][0:M, gi_, half * 512:(half + 1) * 512], gi_ == 0, gi_ == len(EG) - 1,
                         r=["gt%d" % s_, "yr%d" % s_], w=["psc%d" % half])
            if c + 1 < NT:
                prep6(c + 1)
            for half in range(2):
                hs = slice(half * 512, (half + 1) * 512)
                k.tt("vector", hh_[s_][:, hs], hh_[s_][:, hs], PS[:, 2 + half, :], ALU.add, r=["psc%d" % half, "hh%d" % s_], w=["hh%d" % s_])
            k.act(junk6, hh_[s_], AF.Square, r=["hh%d" % s_], w=["junk6", "ss6_%d" % s_], accum_out=st6[:, 2 * c:2 * c + 1])
            rstd(st6[:, 2 * c + 1:2 * c + 2], st6[:, 2 * c:2 * c + 1], DM, ["ss6_%d" % s_], ["r6_%d" % s_])
            k.stt("vector", ob[s_], hh_[s_], st6[:, 2 * c + 1:2 * c + 2], gfbc[:], ALU.mult, ALU.mult,
                  r=["hh%d" % s_, "r6_%d" % s_, "gfbc"], w=["ob%d" % s_])
            k.dma("sync", out_d[tok, :], ob[s_], r=["ob%d" % s_])
    k.barrier()

    sems = {}
    pools = {}
    import contextlib
    with contextlib.ExitStack() as es:
        for e in ENGS:
            sems[e] = es.enter_context(nc.semaphore("s_" + e))
            pools[e] = [es.enter_context(nc.semaphore("d_%s%d" % (e, i))) for i in range(NPOOL)]
        k.emit(sems, pools)
    return nc


def _consts():
    ident = np.eye(128, dtype=np.float32)
    i = np.arange(128)[:, None]; j = np.arange(128)[None, :]
    triu = (i < j).astype(np.float32)
    mA = (j <= i).astype(np.float32)
    mB = (j >= i).astype(np.float32)
    mask = np.concatenate([mA, mB, mA, mB], axis=1)
    return ident, triu, mask


def _tables():
    def inv_freq(dim):
        return (1.0 / (10000.0 ** (np.arange(0, dim, 2, dtype=np.float32) / np.float32(dim)))).astype(np.float32)
    t = np.arange(S, dtype=np.float32)
    row = np.floor(t / 64).astype(np.float32); col = (t % 64).astype(np.float32)
    f = inv_freq(32)
    angA = np.concatenate([row[:, None] * f, col[:, None] * f], axis=-1).astype(np.float32)
    angB = (t[:, None] * inv_freq(64)).astype(np.float32)
    return np.concatenate([np.cos(angA), np.sin(angA), np.cos(angB), np.sin(angB)], axis=-1).astype(np.float32)


def make_in_maps(inp, cores):
    ident, triu, mask = _consts()
    tabs = _tables()
    f = lambda a: np.ascontiguousarray(a, dtype=np.float32)
    shared = {
        "w_in": f(inp["w_in"][0]), "w_out": f(inp["w_out"][0]),
        "g1": f(inp["norm1_g"][0].reshape(8, 128).T),
        "gab": f(np.concatenate([inp["out_norm_a_g"][0], inp["out_norm_b_g"][0]]).reshape(8, 128).T),
        "g2": f(inp["norm2_g"][0]), "gf": f(inp["final_norm_g"]),
        "gq": f(inp["q_norm_g"][0]), "gk": f(inp["k_norm_g"][0]),
        "rw": f(np.concatenate([inp["router_group_w"][0], inp["router_expert_w"][0]], axis=1)),
        "rb": f(np.concatenate([inp["router_group_b"][0], inp["router_expert_b"][0]])),
        "w_gate": f(inp["w_gate"][0]), "w_up": f(inp["w_up"][0]), "w_down": f(inp["w_down"][0]),
        "tabs": tabs, "c_ident": ident, "c_triu": triu, "c_mask": mask,
        "c_e1024": (np.arange(NE) * CAP).astype(np.float32),
        "c_iota": np.arange(CT).astype(np.float32),
    }
    maps = []
    for c in cores:
        m = dict(shared)
        xb = f(inp["x"][c])
        m["x"] = xb
        maps.append(m)
    return maps


def kernel(**inputs):
    inp = {k_: np.asarray(v) for k_, v in inputs.items()}
    nc = build_nc(debug=False)
    maps = make_in_maps(inp, list(range(8)))
    res = run_bass_kernel_spmd(nc, maps, core_ids=list(range(8)))
    out = np.stack([np.asarray(r["out"]) for r in res.results], axis=0)
    return out.astype(np.float32)
```
